# Optimizing a Trainium2 kernel written in Bass

```python
import math
import jax, jax.numpy as jnp
from jax import lax
import numpy as np

D_MODEL = 1024
BATCH = 16
SEQ = 4096
DEPTH = 2

GRID_W = 64
CTX_LEN = 256
D_MIX = D_MODEL
D_ATT = D_MIX // 2
D_CONV = D_MIX - D_ATT
N_HEADS = 4
DV = D_ATT // N_HEADS
DQK = DV // 2
N_FREQ = DQK // 4
ROPE_THETA = 10000.0
Q_BLOCK = 128
N_EXPERTS = 16
N_GROUPS = 4
EXPERTS_PER_GROUP = N_EXPERTS // N_GROUPS
TOP_K = 2
D_EXPERT = 512
LN_EPS = 1e-6
HEAD_NORM_EPS = 1e-5
ALPHA = (2 * DEPTH) ** 0.25
BETA = (8 * DEPTH) ** -0.25

kernel_name = 'hybrid_diffattn_shortconv_grouped_moe_dit'


def layer_norm(x, g=None, b=None):
    xf = x.astype(jnp.float32)
    mu = jnp.mean(xf, axis=-1, keepdims=True)
    var = jnp.mean(jnp.square(xf - mu), axis=-1, keepdims=True)
    y = ((xf - mu) * lax.rsqrt(var + LN_EPS)).astype(x.dtype)
    if g is not None:
        y = y * g + b
    return y


def modulate(x, shift, scale):
    return layer_norm(x) * (1 + scale) + shift


def rope_tables(n):
    t = jnp.arange(n, dtype=jnp.int32)
    row = (t // GRID_W).astype(jnp.float32)
    col = (t % GRID_W).astype(jnp.float32)
    inv = 1.0 / (ROPE_THETA ** (jnp.arange(N_FREQ, dtype=jnp.float32) / N_FREQ))
    ang = jnp.stack([row[:, None] * inv, col[:, None] * inv], axis=1)
    return jnp.cos(ang), jnp.sin(ang)


def apply_rope(x, cos, sin):
    xr = x.reshape(x.shape[:-1] + (2, 2, N_FREQ))
    x1, x2 = xr[..., 0, :], xr[..., 1, :]
    c = cos[None, :, None, None].astype(x.dtype)
    s = sin[None, :, None, None].astype(x.dtype)
    out = jnp.stack([x1 * c - x2 * s, x2 * c + x1 * s], axis=-2)
    return out.reshape(x.shape)


def diff_attend(q, k, v, lam):
    s = jnp.einsum('bqhmd,bkhmd->bhmqk', q, k).astype(jnp.float32) * (DQK ** -0.5)
    p = jax.nn.softmax(s, axis=-1)
    a = (p[:, :, 0] - lam * p[:, :, 1]).astype(v.dtype)
    return jnp.einsum('bhqk,bkhe->bqhe', a, v)


def diff_attend_blocked(q, k, v, lam):
    b, n = q.shape[0], q.shape[1]
    nb = n // Q_BLOCK
    qb = q.reshape(b, nb, Q_BLOCK, N_HEADS, 2, DQK).transpose(1, 0, 2, 3, 4, 5)
    ob = lax.map(lambda blk: diff_attend(blk, k, v, lam), qb)
    return ob.transpose(1, 0, 2, 3, 4).reshape(b, n, N_HEADS, DV)


def head_rmsnorm(o, g):
    of = o.astype(jnp.float32)
    y = of * lax.rsqrt(jnp.mean(jnp.square(of), axis=-1, keepdims=True) + HEAD_NORM_EPS)
    return y.astype(o.dtype) * g.reshape(N_HEADS, DV)


def short_conv(gb, gc, gx, w, b):
    g = gc * gx
    gp = jnp.pad(g, ((0, 0), (1, 1), (0, 0)))
    y = w[0] * gp[:, :-2] + w[1] * gp[:, 1:-1] + w[2] * gp[:, 2:] + b
    return gb * y


def split_heads(q, k, v):
    b, n = q.shape[0], q.shape[1]
    return (q.reshape(b, n, N_HEADS, 2, DQK), k.reshape(b, n, N_HEADS, 2, DQK),
            v.reshape(b, n, N_HEADS, DV))


def mixer(u, uc, w_in, lam, lam_init, attn_g, conv_w, conv_b, w_out, cos, sin, ctx_out):
    b, n, _ = u.shape
    q, k, v, gb, gc, gx = jnp.split(u @ w_in, 6, axis=-1)
    q, k, v = split_heads(q, k, v)
    q = apply_rope(q, cos, sin)
    k = apply_rope(k, cos, sin)
    if ctx_out:
        qc, kc, vc, gbc, gcc, gxc = jnp.split(uc @ w_in, 6, axis=-1)
        qc, kc, vc = split_heads(qc, kc, vc)
    else:
        kc, vc = jnp.split(uc @ w_in[:, D_ATT:3 * D_ATT], 2, axis=-1)
        lc = kc.shape[1]
        kc = kc.reshape(b, lc, N_HEADS, 2, DQK)
        vc = vc.reshape(b, lc, N_HEADS, DV)
    k_all = jnp.concatenate([k, kc], axis=1)
    v_all = jnp.concatenate([v, vc], axis=1)
    att = diff_attend_blocked(q, k_all, v_all, lam)
    att = (head_rmsnorm(att, attn_g) * (1.0 - lam_init)).reshape(b, n, D_ATT)
    conv = short_conv(gb, gc, gx, conv_w, conv_b)
    y = jnp.concatenate([att, conv], axis=-1) @ w_out
    if not ctx_out:
        return y, None
    lc = qc.shape[1]
    att_c = diff_attend(qc, kc, vc, lam)
    att_c = (head_rmsnorm(att_c, attn_g) * (1.0 - lam_init)).reshape(b, lc, D_ATT)
    conv_c = short_conv(gbc, gcc, gxc, conv_w, conv_b)
    yc = jnp.concatenate([att_c, conv_c], axis=-1) @ w_out
    return y, yc


def moe(h, router_w, router_bias, w_gate, w_up, w_down):
    shp = h.shape
    t = h.reshape(-1, shp[-1])
    scores = jax.nn.sigmoid((t @ router_w).astype(jnp.float32))
    sel = scores + router_bias.astype(jnp.float32)
    grp = sel.reshape(-1, N_GROUPS, EXPERTS_PER_GROUP)
    gscore = lax.top_k(grp, TOP_K)[0].sum(-1)
    best = jnp.argmax(gscore, axis=-1)
    in_group = (jnp.arange(N_EXPERTS) // EXPERTS_PER_GROUP)[None, :] == best[:, None]
    masked = jnp.where(in_group, sel, -jnp.inf)
    _, idx = lax.top_k(masked, TOP_K)
    w = jnp.take_along_axis(scores, idx, axis=-1)
    w = w / jnp.sum(w, axis=-1, keepdims=True)
    gates = jnp.sum(jax.nn.one_hot(idx, N_EXPERTS, dtype=jnp.float32) * w[..., None], axis=1).astype(t.dtype)
    out = jnp.zeros_like(t)
    for e in range(N_EXPERTS):
        he = jax.nn.silu(t @ w_gate[e]) * (t @ w_up[e])
        out = out + gates[:, e:e + 1] * (he @ w_down[e])
    return out.reshape(shp)


def setup_inputs(seed: int = 0) -> dict:
    key = jax.random.key(seed)
    ks = jax.random.split(key, 24)
    nrm = lambda k, s, sc: jax.random.normal(k, s, jnp.float32) * sc
    D = D_MODEL
    return {
        'x': nrm(ks[0], (BATCH, SEQ, D), 1.0),
        'c': nrm(ks[1], (BATCH, D), 1.0),
        'ctx': nrm(ks[2], (BATCH, CTX_LEN, D), 1.0),
        'c_ctx': nrm(ks[3], (D,), 1.0),
        'w_mod': nrm(ks[4], (DEPTH, D, 6 * D), 0.5 * D ** -0.5),
        'b_mod': nrm(ks[5], (DEPTH, 6 * D), 0.02),
        'w_in': nrm(ks[6], (DEPTH, D, 3 * D_ATT + 3 * D_CONV), D ** -0.5),
        'diff_lambda': nrm(ks[7], (DEPTH, 4, DQK), 0.1),
        'attn_norm_g': 1.0 + nrm(ks[8], (DEPTH, D_ATT), 0.02),
        'conv_w': nrm(ks[9], (DEPTH, 3, D_CONV), 3 ** -0.5),
        'conv_b': nrm(ks[10], (DEPTH, D_CONV), 0.02),
        'w_out': nrm(ks[11], (DEPTH, D_MIX, D), BETA * D_MIX ** -0.5),
        'ln1_g': 1.0 + nrm(ks[12], (DEPTH, D), 0.02),
        'ln1_b': nrm(ks[13], (DEPTH, D), 0.02),
        'ln2_g': 1.0 + nrm(ks[14], (DEPTH, D), 0.02),
        'ln2_b': nrm(ks[15], (DEPTH, D), 0.02),
        'router_w': nrm(ks[16], (D, N_EXPERTS), D ** -0.5),
        'router_bias': nrm(ks[17], (N_EXPERTS,), 0.01),
        'w_gate': nrm(ks[18], (DEPTH, N_EXPERTS, D, D_EXPERT), D ** -0.5),
        'w_up': nrm(ks[19], (DEPTH, N_EXPERTS, D, D_EXPERT), D ** -0.5),
        'w_down': nrm(ks[20], (DEPTH, N_EXPERTS, D_EXPERT, D), BETA * D_EXPERT ** -0.5),
    }


def reference(x, c, ctx, c_ctx, w_mod, b_mod, w_in, diff_lambda, attn_norm_g, conv_w, conv_b, w_out,
              ln1_g, ln1_b, ln2_g, ln2_b, router_w, router_bias, w_gate, w_up, w_down):
    n_lat = x.shape[1]
    cos, sin = rope_tables(n_lat)
    for l in range(DEPTH):
        last = l == DEPTH - 1
        m = (jax.nn.silu(c) @ w_mod[l] + b_mod[l])[:, None, :]
        sh1, sc1, g1, sh2, sc2, g2 = jnp.split(m, 6, axis=-1)
        mc = jax.nn.silu(c_ctx) @ w_mod[l] + b_mod[l]
        sh1c, sc1c, g1c, sh2c, sc2c, g2c = jnp.split(mc, 6, axis=-1)
        lam_init = 0.8 - 0.6 * math.exp(-0.3 * l)
        lp = diff_lambda[l].astype(jnp.float32)
        lam = jnp.exp(jnp.dot(lp[0], lp[1])) - jnp.exp(jnp.dot(lp[2], lp[3])) + lam_init
        u = modulate(x, sh1, sc1)
        uc = modulate(ctx, sh1c, sc1c)
        mix_x, mix_c = mixer(u, uc, w_in[l], lam, lam_init, attn_norm_g[l], conv_w[l], conv_b[l], w_out[l],
                             cos, sin, not last)
        x = layer_norm(ALPHA * x + g1 * mix_x, ln1_g[l], ln1_b[l])
        if last:
            f = moe(modulate(x, sh2, sc2), router_w, router_bias, w_gate[l], w_up[l], w_down[l])
            x = layer_norm(ALPHA * x + g2 * f, ln2_g[l], ln2_b[l])
        else:
            ctx = layer_norm(ALPHA * ctx + g1c * mix_c, ln1_g[l], ln1_b[l])
            lc = ctx.shape[1]
            h = jnp.concatenate([modulate(ctx, sh2c, sc2c), modulate(x, sh2, sc2)], axis=1)
            f = moe(h, router_w, router_bias, w_gate[l], w_up[l], w_down[l])
            ctx = layer_norm(ALPHA * ctx + g2c * f[:, :lc], ln2_g[l], ln2_b[l])
            x = layer_norm(ALPHA * x + g2 * f[:, lc:], ln2_g[l], ln2_b[l])
    return x
```

```python
import math
from contextlib import ExitStack

import numpy as np

import concourse.bass as bass
import concourse.mybir as mybir
from concourse.bass_utils import run_bass_kernel_spmd

F32 = mybir.dt.float32
BF16 = mybir.dt.bfloat16
AF = mybir.ActivationFunctionType
ALU = mybir.AluOpType

P = 128
D = 1024
KD = 8
NLAT = 4096
NCTX = 256
NTOK = NLAT + NCTX
NT = NTOK // P
NH = 4
NE = 16
DEXP = 512
DEPTH = 2
ALPHA = (2 * DEPTH) ** 0.25
LN_EPS = 1e-6
HEAD_EPS = 1e-5
QSCALE = 64 ** -0.5
NCORES = 8
BIG = 100.0


class Res:
    __slots__ = ("name", "w", "r", "excl")

    def __init__(self, name, excl=False):
        self.name = name
        self.w = None
        self.r = {}
        self.excl = excl


class Stream:
    __slots__ = ("sem", "count")

    def __init__(self, sem):
        self.sem = sem
        self.count = 0


class EngQ:
    def __init__(self, name, sem, inorder):
        self.name = name
        self.sem = sem
        self.count = 0
        self.waited = {}
        self.ops = []
        self.inorder = inorder


class Sched:
    def __init__(self, nc, es):
        self.nc = nc
        self.es = es
        self.q = {}
        for name, inorder in (("pe", True), ("act", False), ("dve", False), ("pool", False), ("sp", True)):
            sem = es.enter_context(nc.semaphore("q_" + name))
            self.q[name] = EngQ(name, sem, inorder)
        self.streams = []

    def stream(self):
        s = Stream(self.es.enter_context(self.nc.semaphore("d%d" % len(self.streams))))
        self.streams.append(s)
        return s

    def _emit(self, eng, fn, reads, writes, stream):
        E = self.q[eng]
        waits = {}

        def need(sv):
            if sv is None:
                return
            s, v = sv
            if s is E.sem and E.inorder and stream is None:
                return
            k = id(s)
            if E.waited.get(k, 0) >= v:
                return
            if k not in waits or waits[k][1] < v:
                waits[k] = (s, v)

        for r in reads:
            need(r.w)
            if r.excl:
                for sv in r.r.values():
                    if sv[0] is not E.sem:
                        need(sv)
        for w in writes:
            need(w.w)
            for sv in w.r.values():
                need(sv)
        if stream is not None and stream.count:
            need((stream.sem, stream.count))
        for k, (s, v) in waits.items():
            E.waited[k] = v
        if stream is None:
            E.count += 1
            done = (E.sem, E.count)
            inc = 1
        else:
            stream.count += 16
            done = (stream.sem, stream.count)
            inc = 16
        E.ops.append((list(waits.values()), fn, done[0], inc))
        k = id(done[0])
        for r in reads:
            r.r[k] = done
        for w in writes:
            w.w = done
            w.r = {}
        return done

    def op(self, eng, fn, reads=(), writes=()):
        return self._emit(eng, fn, reads, writes, None)

    def dma(self, eng, stream, out, in_, reads=(), writes=()):
        return self._emit(eng, lambda e: e.dma_start(out=out, in_=in_), reads, writes, stream)

    def barrier(self, engines=("pe", "act", "dve", "pool", "sp")):
        targets = [(E.sem, E.count) for E in self.q.values() if E.count]
        targets += [(s.sem, s.count) for s in self.streams if s.count]
        for name in engines:
            E = self.q[name]
            waits = []
            for s, v in targets:
                if s is E.sem and E.inorder:
                    continue
                k = id(s)
                if E.waited.get(k, 0) >= v:
                    continue
                E.waited[k] = v
                waits.append((s, v))
            if waits:
                E.ops.append((waits, None, None, 0))

    def replay(self, block):
        def run(E):
            def body(eng):
                for waits, fn, sem, inc in E.ops:
                    for s, v in waits:
                        eng.wait_ge(s, v)
                    if fn is not None:
                        fn(eng).then_inc(sem, inc)
            return body

        block.tensor(run(self.q["pe"]))
        block.scalar(run(self.q["act"]))
        block.vector(run(self.q["dve"]))
        block.gpsimd(run(self.q["pool"]))
        block.sync(run(self.q["sp"]))


class Buf:
    __slots__ = ("ap", "r", "st")

    def __init__(self, ap, r, st=None):
        self.ap = ap
        self.r = r
        self.st = st


class Builder:
    def __init__(self, layers=(0, 1), debug=None, nbatch=2, stop_after=None, scratch_out=False, nb_run=None):
        self.layers = tuple(layers)
        self.debug = debug or {}
        self.nbatch = nbatch
        self.stop_after = stop_after
        self.scratch_out = scratch_out
        self.nb_run = nb_run if nb_run is not None else nbatch

    def mm(self, out, lhsT, rhs, start, stop, R, W, skip=False):
        self.S.op("pe", lambda e: e.matmul(out, lhsT=lhsT, rhs=rhs, start=start, stop=stop,
                                            skip_group_check=skip), reads=R, writes=W)

    def tr(self, out, in_, ident, R, W):
        self.S.op("pe", lambda e: e.transpose(out, in_, ident), reads=R, writes=W)

    def act(self, out, in_, func, R, W, scale=1.0, bias=0.0):
        self.S.op("act", lambda e: e.activation(out=out, in_=in_, func=func, bias=bias, scale=scale),
                  reads=R, writes=W)

    def tt(self, eng, out, in0, in1, op, R, W):
        self.S.op(eng, lambda e: e.tensor_tensor(out=out, in0=in0, in1=in1, op=op), reads=R, writes=W)

    def ts(self, eng, out, in0, s1, s2, op0, op1, R, W):
        if s2 is None:
            self.S.op(eng, lambda e: e.tensor_scalar(out=out, in0=in0, scalar1=s1, scalar2=None, op0=op0),
                      reads=R, writes=W)
        else:
            self.S.op(eng, lambda e: e.tensor_scalar(out=out, in0=in0, scalar1=s1, scalar2=s2, op0=op0, op1=op1),
                      reads=R, writes=W)

    def stt(self, eng, out, in0, scalar, in1, op0, op1, R, W):
        self.S.op(eng, lambda e: e.scalar_tensor_tensor(out=out, in0=in0, scalar=scalar, in1=in1, op0=op0, op1=op1),
                  reads=R, writes=W)

    def cp(self, eng, out, in_, R, W):
        self.S.op(eng, lambda e: e.tensor_copy(out=out, in_=in_), reads=R, writes=W)

    def red(self, eng, out, in_, op, R, W):
        self.S.op(eng, lambda e: e.tensor_reduce(out=out, in_=in_, axis=mybir.AxisListType.X, op=op),
                  reads=R, writes=W)

    def memset(self, eng, ap, val, W):
        self.S.op(eng, lambda e: e.memset(ap, val), writes=W)

    def dma(self, eng, buf_or_stream, out, in_, R, W):
        st = buf_or_stream.st if isinstance(buf_or_stream, Buf) else buf_or_stream
        self.S.dma(eng, st, out, in_, reads=R, writes=W)

    def sb(self, name, shape, dtype, stream=False):
        t = self.es.enter_context(self.nc.sbuf_tensor("s_" + name, list(shape), dtype))
        return Buf(t[:], Res(name), self.S.stream() if stream else None)

    def arena_reset(self):
        self.aoff = 0

    def aalloc(self, name, free_shape, dtype, stream=False):
        n = 1
        for s in free_shape:
            n *= s
        nb16 = n * (2 if dtype == F32 else 1)
        nb16 = (nb16 + 15) // 16 * 16
        assert self.aoff + nb16 <= self.asize, (name, self.aoff, nb16, self.asize)
        v = self.arena[:, self.aoff:self.aoff + n * (2 if dtype == F32 else 1)]
        self.aoff += nb16
        if dtype == F32:
            v = v.bitcast(F32)
        if len(free_shape) == 2:
            v = v.rearrange("p (a b) -> p a b", a=free_shape[0])
        elif len(free_shape) == 3:
            v = v.rearrange("p (a b c) -> p a b c", a=free_shape[0], b=free_shape[1])
        st = None
        if stream:
            if self.spool_i >= len(self.spool):
                self.spool.append(self.S.stream())
            st = self.spool[self.spool_i]
            self.spool_i += 1
        return Buf(v, Res(name), st)

    def phase_begin(self):
        self.S.barrier()
        self.arena_reset()
        self.spool_i = 0

    def build(self):
        nc = bass.Bass("TRN2", target_bir_lowering=False)
        self.nc = nc
        NB = self.nbatch
        din = lambda name, shape, dt=F32: nc.dram_tensor(name, list(shape), dt, kind="ExternalInput").ap()
        self.x_in = din("x", [NB, NLAT, D])
        self.ctx_in = din("ctx", [NB, NCTX, D])
        self.ccT_in = din("ccT", [P, KD, 3])
        self.w_mod = din("w_mod", [DEPTH, D, 6 * D])
        self.b_mod = din("b_mod", [DEPTH, 6 * D])
        self.w_inx = din("w_inx", [DEPTH, D, 4096])
        self.dl_in = din("dl", [1, DEPTH * 4 * 64])
        self.gT_in = din("gT", [P, DEPTH, NH])
        self.cwT_in = din("cwT", [P, DEPTH, 3, 4])
        self.cbT_in = din("cbT", [P, DEPTH, 4])
        self.w_out = din("w_out", [DEPTH, D, D])
        self.ln1_g = din("ln1_g", [DEPTH, D])
        self.ln1_b = din("ln1_b", [DEPTH, D])
        self.ln2_g = din("ln2_g", [DEPTH, D])
        self.ln2_b = din("ln2_b", [DEPTH, D])
        self.router_w = din("router_w", [D, NE])
        self.router_bias = din("router_bias", [1, NE])
        self.w_gate = din("w_gate", [DEPTH, NE, D, DEXP])
        self.w_up = din("w_up", [DEPTH, NE, D, DEXP])
        self.w_down = din("w_down", [DEPTH, NE, DEXP, D])
        self.ident_in = din("ident", [P, P])
        self.cos_in = din("cosT", [P, NLAT])
        self.sin_in = din("sinT", [P, NLAT])
        self.y = nc.dram_tensor("y", [NB, NLAT, D], F32, kind="ExternalOutput").ap()
        self.dbg_out = {}
        for name, shape in self.debug.items():
            self.dbg_out[name] = nc.dram_tensor("dbg_" + name, list(shape), F32, kind="ExternalOutput").ap()
        sk = {"kind": "ExternalOutput"} if self.scratch_out else {}
        self.xs_d = nc.dram_tensor("xs_d", [NB, NTOK, D], F32, **sk).ap()
        self.QT_d = nc.dram_tensor("QT_d", [NH, P, NTOK], BF16, **sk).ap()
        self.KT_d = nc.dram_tensor("KT_d", [NH, P, NTOK], BF16, **sk).ap()
        self.VS_d = nc.dram_tensor("VS_d", [NTOK, 512], BF16, **sk).ap()
        self.CV_d = nc.dram_tensor("CV_d", [4, P, NTOK], BF16, **sk).ap()
        self.mrow_d = nc.dram_tensor("mrow_d", [DEPTH, 3, 6 * D], F32, **sk).ap()
        self.xs_r = [[Res("xs%d_%d" % (b, i)) for i in range(NT)] for b in range(NB)]
        self.QT_r = [[Res("qt%d_%d" % (h, j)) for j in range(9)] for h in range(NH)]
        self.KT_r = [[Res("kt%d_%d" % (h, j)) for j in range(9)] for h in range(NH)]
        self.VS_r = [Res("vs%d" % i) for i in range(NT)]
        self.CV_r = [[Res("cv%d_%d" % (c, j)) for j in range(3)] for c in range(4)]
        self.mrow_r = [Res("mrowd%d" % l) for l in range(DEPTH)]
        self.y_r = Res("y")
        self.dbg_r = Res("dbg")

        with ExitStack() as es:
            self.es = es
            self.S = Sched(nc, es)
            S = self.S
            self.PB = []
            for i in range(8):
                t = es.enter_context(nc.psum_tensor("pb%d" % i, [P, 512], F32))
                self.PB.append(Buf(t[:], Res("pb%d" % i, excl=True)))
            self.identf = self.sb("identf", [P, P], F32, stream=True)
            self.identb = self.sb("identb", [P, P], BF16)
            self.onesb = self.sb("onesb", [P, P], BF16)
            self.onesf = self.sb("onesf", [P, P], F32)
            self.scT = self.sb("scT", [P, KD, 3], F32, stream=True)
            self.mT = self.sb("mT", [P, DEPTH, 48, 3], F32)
            self.m1p = self.sb("m1p", [P, DEPTH, 2, KD, 3], F32)
            self.dl = self.sb("dl", [P, DEPTH * 4 * 64], F32, stream=True)
            self.lamt = self.sb("lamt", [P, 8], F32)
            self.nlam = self.sb("nlam", [P, DEPTH], F32)
            self.gT = self.sb("gTs", [P, DEPTH, NH], F32, stream=True)
            self.gsc = self.sb("gsc", [P, DEPTH, NH], F32)
            self.cw = self.sb("cw", [P, DEPTH, 3, 4], F32, stream=True)
            self.cb = self.sb("cbs", [P, DEPTH, 4], F32, stream=True)
            self.rwb = self.sb("rwb", [P, KD, NE], BF16, stream=True)
            self.rbias = self.sb("rbias", [P, NE], F32, stream=True)
            self.stat = [self.sb("stat%d" % i, [P, 16], F32) for i in range(3)]
            self.rt = [self.sb("rt%d" % i, [P, 16 * 12], F32) for i in range(2)]
            self.asize = 99 * 1024
            arena_t = es.enter_context(nc.sbuf_tensor("arena", [P, self.asize], BF16))
            self.arena = arena_t[:]
            self.spool = []
            self.spool_i = 0
            self.arena_reset()
            self.dbg_stream = S.stream()

            self.setup_phase()
            first = True
            stop = self.stop_after
            done = False
            for l in self.layers:
                last = l == DEPTH - 1
                self.mod_phase(l)
                if stop == "mod":
                    break
                for b in range(self.nb_run):
                    self.lnt_phase_and_inproj(l, b, first, last)
                    if stop in ("lnt", "inproj"):
                        done = True
                        break
                    self.att_phase(l, b, first, last)
                    if stop in ("att", "att_q0", "att_q0b", "att_q1b"):
                        done = True
                        break
                    self.moe_phase(l, b, last)
                if done:
                    break
                first = False
            S.barrier()
            with nc.Block() as block:
                S.replay(block)
        return nc

    def xsrc(self, first, b, i):
        if first:
            if i < 32:
                return self.x_in[b, i * P:(i + 1) * P, :], None
            return self.ctx_in[b, (i - 32) * P:(i - 31) * P, :], None
        return self.xs_d[b, i * P:(i + 1) * P, :], self.xs_r[b][i]

    def setup_phase(self):
        S = self.S
        self.dma("sp", self.identf, self.identf.ap, self.ident_in, [], [self.identf.r])
        self.cp("dve", self.identb.ap, self.identf.ap, [self.identf.r], [self.identb.r])
        self.memset("pool", self.onesb.ap, 1.0, [self.onesb.r])
        self.memset("pool", self.onesf.ap, 1.0, [self.onesf.r])
        self.dma("sp", self.scT, self.scT.ap, self.ccT_in, [], [self.scT.r])
        self.act(self.scT.ap, self.scT.ap, AF.Silu, [self.scT.r], [self.scT.r])
        self.dma("sp", self.dl, self.dl.ap, self.dl_in.broadcast_to([P, DEPTH * 4 * 64]), [], [self.dl.r])
        self.dma("sp", self.gT, self.gT.ap, self.gT_in, [], [self.gT.r])
        self.dma("sp", self.cw, self.cw.ap, self.cwT_in, [], [self.cw.r])
        self.dma("sp", self.cb, self.cb.ap, self.cbT_in, [], [self.cb.r])
        self.dma("pool", self.rwb, self.rwb.ap, self.router_w.rearrange("(k p) e -> p k e", p=P), [], [self.rwb.r])
        self.dma("sp", self.rbias, self.rbias.ap, self.router_bias.broadcast_to([P, NE]), [], [self.rbias.r])
        dl4 = self.dl.ap.rearrange("p (l j d) -> p l j d", l=DEPTH, j=4)
        lt = self.lamt
        for l in range(DEPTH):
            lam_init = 0.8 - 0.6 * math.exp(-0.3 * l)
            tmp = self.rt[0]
            for j in range(2):
                self.tt("dve", tmp.ap[:, 0:64], dl4[:, l, 2 * j, :], dl4[:, l, 2 * j + 1, :], ALU.mult,
                        [self.dl.r], [tmp.r])
                self.red("dve", lt.ap[:, 4 * l + j:4 * l + j + 1], tmp.ap[:, 0:64], ALU.add, [tmp.r], [lt.r])
            self.act(lt.ap[:, 4 * l:4 * l + 2], lt.ap[:, 4 * l:4 * l + 2], AF.Exp, [lt.r], [lt.r])
            self.tt("dve", lt.ap[:, 4 * l + 2:4 * l + 3], lt.ap[:, 4 * l + 1:4 * l + 2], lt.ap[:, 4 * l:4 * l + 1],
                    ALU.subtract, [lt.r], [lt.r])
            self.ts("dve", self.nlam.ap[:, l:l + 1], lt.ap[:, 4 * l + 2:4 * l + 3], -lam_init, None, ALU.add, None,
                    [lt.r], [self.nlam.r])
            self.ts("dve", self.gsc.ap[:, l, :], self.gT.ap[:, l, :], 1.0 - lam_init, None, ALU.mult, None,
                    [self.gT.r], [self.gsc.r])

    def mod_phase(self, l):
        self.phase_begin()
        wm = [self.aalloc("wm%d" % i, [KD, 512], F32, stream=True) for i in range(2)]
        mrow = self.aalloc("mrow", [6 * D], F32, stream=True)
        bm3 = self.aalloc("bm3", [6 * D], F32, stream=True)
        self.dma("sp", bm3, bm3.ap[0:3, :], self.b_mod[l:l + 1, :].broadcast_to([3, 6 * D]), [], [bm3.r])
        for cb in range(12):
            w = wm[cb % 2]
            self.dma("sp", w, w.ap, self.w_mod[l, :, cb * 512:(cb + 1) * 512].rearrange("(k p) n -> p k n", p=P),
                     [], [w.r])
            bank = self.PB[cb % 2]
            for k in range(KD):
                self.mm(bank.ap[0:3, :], self.scT.ap[:, k, :], w.ap[:, k, :], k == 0, k == KD - 1,
                        [self.scT.r, w.r], [bank.r])
            self.tt("dve", mrow.ap[0:3, cb * 512:(cb + 1) * 512], bank.ap[0:3, :], bm3.ap[0:3, cb * 512:(cb + 1) * 512],
                    ALU.add, [bank.r, bm3.r], [mrow.r])
        self.dma("sp", mrow, self.mrow_d[l], mrow.ap[0:3, :], [mrow.r], [self.mrow_r[l]])
        pT = self.PB[2]
        pTv = pT.ap[:, 0:144].rearrange("p (j c) -> p j c", c=3)
        for jn in range(48):
            self.tr(pTv[:, jn, :], mrow.ap[0:3, jn * P:(jn + 1) * P], self.identf.ap[0:3, 0:3],
                    [mrow.r, self.identf.r], [pT.r])
        self.cp("dve", self.mT.ap[:, l, :, :], pTv, [pT.r], [self.mT.r])
        for w_, v in ((0, 1), (1, 4)):
            self.ts("dve", self.m1p.ap[:, l, w_, :, :], self.mT.ap[:, l, v * 8:(v + 1) * 8, :], 1.0, None, ALU.add, None,
                    [self.mT.r], [self.m1p.r])

    def lnt_setup(self):
        self.lnt_xt = [self.aalloc("lxt%d" % i, [D], F32, stream=True) for i in range(3)]
        self.lnt_xn = [self.aalloc("lxn%d" % i, [D], BF16) for i in range(3)]
        self.lnt_cnt = 0

    def ln_stats(self, z, st):
        self.S.op("dve", lambda e: e.bn_stats(st.ap[:, 0:6], z.ap[:, 0:512]), reads=[z.r], writes=[st.r])
        self.S.op("dve", lambda e: e.bn_stats(st.ap[:, 6:12], z.ap[:, 512:1024]), reads=[z.r], writes=[st.r])
        self.S.op("dve", lambda e: e.bn_aggr(st.ap[:, 12:14], st.ap[:, 0:12]), reads=[st.r], writes=[st.r])
        self.act(st.ap[:, 14:15], st.ap[:, 13:14], AF.Ln, [st.r], [st.r], bias=LN_EPS)
        self.act(st.ap[:, 14:15], st.ap[:, 14:15], AF.Exp, [st.r], [st.r], scale=-0.5)
        self.stt("dve", st.ap[:, 15:16], st.ap[:, 12:13], -1.0, st.ap[:, 14:15], ALU.mult, ALU.mult, [st.r], [st.r])

    def lnt_tile(self, src_ap, src_r, UT, tok_off, scp, shv):
        c = self.lnt_cnt
        self.lnt_cnt += 1
        bi = c % 3
        xt, xn, st = self.lnt_xt[bi], self.lnt_xn[bi], self.stat[bi]
        self.dma("sp", xt, xt.ap, src_ap, [src_r] if src_r is not None else [], [xt.r])
        self.ln_stats(xt, st)
        self.ts("dve", xn.ap, xt.ap, st.ap[:, 12:13], st.ap[:, 14:15], ALU.subtract, ALU.mult, [xt.r, st.r], [xn.r])
        pa, pb = self.PB[2 * bi], self.PB[2 * bi + 1]
        for half, bank in ((0, pa), (1, pb)):
            pv = bank.ap.bitcast(BF16).rearrange("p (k t) -> p k t", k=8)
            for kk in range(4):
                k = half * 4 + kk
                self.tr(pv[:, kk, :], xn.ap[:, k * P:(k + 1) * P], self.identb.ap, [xn.r, self.identb.r], [bank.r])
            for kk in range(4):
                k = half * 4 + kk
                dst = UT.ap[:, k, tok_off:tok_off + P]
                if half == 0:
                    self.act(dst, pv[:, kk, :], AF.Identity, [bank.r, self.m1p.r, self.mT.r], [UT.r],
                             scale=scp[:, k:k + 1], bias=shv[:, k:k + 1])
                else:
                    self.ts("dve", dst, pv[:, kk, :], scp[:, k:k + 1], shv[:, k:k + 1], ALU.mult, ALU.add,
                            [bank.r, self.m1p.r, self.mT.r], [UT.r])

    def lnt_phase_and_inproj(self, l, b, first, last):
        self.phase_begin()
        UT = self.aalloc("UT", [KD, NTOK], BF16)
        self.lnt_setup()
        for i in range(NT):
            j = b if i < 32 else 2
            src, src_r = self.xsrc(first, b, i)
            self.lnt_tile(src, src_r, UT, i * P, self.m1p.ap[:, l, 0, :, j], self.mT.ap[:, l, 0:8, j])
        if "uT" in self.dbg_out and l == self.debug_layer and b == 0:
            self.dbg_dump_bf16("uT", UT, KD * NTOK)
        if self.stop_after == "lnt":
            return
        wA = self.aalloc("wA", [KD, 512], BF16, stream=True)
        wB = self.aalloc("wB", [KD, 512], BF16, stream=True)
        wC = self.aalloc("wC", [KD, 512], BF16, stream=True)
        mark = self.aoff
        cosT = self.aalloc("cosT", [NLAT], F32, stream=True)
        sinT = self.aalloc("sinT", [NLAT], F32, stream=True)
        t1 = [self.aalloc("rt1_%d" % i, [512], F32) for i in range(2)]
        t2 = [self.aalloc("rt2_%d" % i, [512], F32) for i in range(2)]
        qo = [self.aalloc("qo%d" % i, [512], BF16, stream=True) for i in range(2)]
        self.dma("sp", cosT, cosT.ap, self.cos_in, [], [cosT.r])
        self.dma("sp", sinT, sinT.ap, self.sin_in, [], [sinT.r])
        wx = self.w_inx[l].rearrange("(k p) n -> p k n", p=P)
        cnt = 0
        for which, base, dst, dst_r in (("q", 0, self.QT_d, self.QT_r), ("k", 1024, self.KT_d, self.KT_r)):
            self.dma("pool", wA, wA.ap, wx[:, :, base:base + 512], [], [wA.r])
            self.dma("pool", wB, wB.ap, wx[:, :, base + 512:base + 1024], [], [wB.r])
            for h in range(NH):
                for jt in range(9):
                    ctx_tile = jt == 8
                    if ctx_tile and which == "q" and last:
                        continue
                    tok0 = jt * 512
                    n = 256 if ctx_tile else 512
                    bi = cnt % 2
                    cnt += 1
                    pa, pb = self.PB[4 + bi], self.PB[6 + bi]
                    for k in range(KD):
                        self.mm(pa.ap[:, 0:n], wA.ap[:, k, h * P:(h + 1) * P], UT.ap[:, k, tok0:tok0 + n],
                                k == 0, k == KD - 1, [wA.r, UT.r], [pa.r])
                    o = qo[bi]
                    if not ctx_tile:
                        for k in range(KD):
                            self.mm(pb.ap[:, 0:n], wB.ap[:, k, h * P:(h + 1) * P], UT.ap[:, k, tok0:tok0 + n],
                                    k == 0, k == KD - 1, [wB.r, UT.r], [pb.r])
                        self.tt("dve", t1[bi].ap, pa.ap, cosT.ap[:, tok0:tok0 + n], ALU.mult, [pa.r, cosT.r], [t1[bi].r])
                        self.tt("dve", t2[bi].ap, pb.ap, sinT.ap[:, tok0:tok0 + n], ALU.mult, [pb.r, sinT.r], [t2[bi].r])
                        self.tt("pool", o.ap, t1[bi].ap, t2[bi].ap, ALU.add, [t1[bi].r, t2[bi].r], [o.r])
                    else:
                        self.act(o.ap[:, 0:n], pa.ap[:, 0:n], AF.Copy, [pa.r], [o.r])
                    self.dma("sp", o, dst[h, :, tok0:tok0 + n], o.ap[:, 0:n], [o.r], [dst_r[h][jt]])
        self.dma("pool", wC, wC.ap, wx[:, :, 2048:2560], [], [wC.r])
        vo = qo
        for i in range(NT):
            bi = i % 2
            pv = self.PB[4 + bi]
            for k in range(KD):
                self.mm(pv.ap, UT.ap[:, k, i * P:(i + 1) * P], wC.ap[:, k, :], k == 0, k == KD - 1, [UT.r, wC.r], [pv.r])
            self.act(vo[bi].ap, pv.ap, AF.Copy, [pv.r], [vo[bi].r])
            self.dma("sp", vo[bi], self.VS_d[i * P:(i + 1) * P, :], vo[bi].ap, [vo[bi].r], [self.VS_r[i]])
        self.S.barrier()
        self.aoff = mark
        GL = NTOK + 8
        G = self.aalloc("G", [GL], F32)
        GB = self.aalloc("GB", [NTOK], F32)
        tmpc = [self.aalloc("tmpc%d" % i, [512], F32) for i in range(2)]
        T = [self.aalloc("T%d" % i, [2048], F32) for i in range(2)]
        Y = [self.aalloc("Y%d" % i, [2048], BF16, stream=True) for i in range(2)]
        self.dma("pool", wA, wA.ap, wx[:, :, 2560:3072], [], [wA.r])
        self.dma("pool", wB, wB.ap, wx[:, :, 3072:3584], [], [wB.r])
        self.dma("pool", wC, wC.ap, wx[:, :, 3584:4096], [], [wC.r])
        self.memset("pool", G.ap[:, 0:1], 0.0, [G.r])
        self.memset("pool", G.ap[:, 4097:4099], 0.0, [G.r])
        self.memset("pool", G.ap[:, 4355:4356], 0.0, [G.r])

        def gidx(t):
            return 1 + t if t < NLAT else 4099 + (t - NLAT)

        ntile = 8 if last else 9
        pieces = [(0, 2048, 0), (2048, 2048, 1)] + ([] if last else [(NLAT, NCTX, 2)])
        ycnt = 0
        for c in range(4):
            for jt in range(ntile):
                tok0 = jt * 512
                n = 256 if jt == 8 else 512
                bi = jt % 2
                pg = [self.PB[bi * 3 + x] for x in range(3)]
                for x, w in enumerate((wA, wB, wC)):
                    for k in range(KD):
                        self.mm(pg[x].ap[:, 0:n], w.ap[:, k, c * P:(c + 1) * P], UT.ap[:, k, tok0:tok0 + n],
                                k == 0, k == KD - 1, [w.r, UT.r], [pg[x].r])
                self.act(GB.ap[:, tok0:tok0 + n], pg[0].ap[:, 0:n], AF.Copy, [pg[0].r], [GB.r])
                self.act(tmpc[bi].ap[:, 0:n], pg[1].ap[:, 0:n], AF.Copy, [pg[1].r], [tmpc[bi].r])
                g0 = gidx(tok0)
                self.tt("dve", G.ap[:, g0:g0 + n], tmpc[bi].ap[:, 0:n], pg[2].ap[:, 0:n], ALU.mult,
                        [tmpc[bi].r, pg[2].r], [G.r])
            for (t0, n, pj) in pieces:
                bi = ycnt % 2
                ycnt += 1
                g0 = gidx(t0)
                Tb, Yb = T[bi], Y[bi]
                self.act(Tb.ap[:, 0:n], G.ap[:, g0:g0 + n], AF.Identity, [G.r, self.cw.r, self.cb.r], [Tb.r],
                         scale=self.cw.ap[:, l, 1, c:c + 1], bias=self.cb.ap[:, l, c:c + 1])
                self.stt("dve", Tb.ap[:, 0:n], G.ap[:, g0 - 1:g0 - 1 + n], self.cw.ap[:, l, 0, c:c + 1], Tb.ap[:, 0:n],
                         ALU.mult, ALU.add, [G.r, self.cw.r, Tb.r], [Tb.r])
                self.stt("dve", Tb.ap[:, 0:n], G.ap[:, g0 + 1:g0 + 1 + n], self.cw.ap[:, l, 2, c:c + 1], Tb.ap[:, 0:n],
                         ALU.mult, ALU.add, [G.r, self.cw.r, Tb.r], [Tb.r])
                self.tt("pool", Yb.ap[:, 0:n], GB.ap[:, t0:t0 + n], Tb.ap[:, 0:n], ALU.mult, [GB.r, Tb.r], [Yb.r])
                self.dma("sp", Yb, self.CV_d[c, :, t0:t0 + n], Yb.ap[:, 0:n], [Yb.r], [self.CV_r[c][pj]])

    def post_ln(self, z, st, zn, o, lng, lnb, dst_ap, dst_r):
        self.ln_stats(z, st)
        self.stt("dve", z.ap, z.ap, st.ap[:, 12:13], lng.ap, ALU.subtract, ALU.mult, [z.r, st.r, lng.r], [z.r])
        self.stt("dve", o.ap, z.ap, st.ap[:, 14:15], lnb.ap, ALU.mult, ALU.add, [z.r, st.r, lnb.r], [o.r])
        self.dma("sp", o, dst_ap, o.ap, [o.r], [dst_r])

    def att_phase(self, l, b, first, last):
        self.phase_begin()
        KT = self.aalloc("KT", [NH, NTOK], BF16, stream=True)
        V = self.aalloc("V", [NT, 512], BF16, stream=True)
        WO = self.aalloc("WO", [KD, D], BF16, stream=True)
        g1bc = {}
        for j in ([b] if last else [b, 2]):
            g1bc[j] = self.aalloc("g1bc%d" % j, [D], F32, stream=True)
            self.dma("sp", g1bc[j], g1bc[j].ap, self.mrow_d[l, j:j + 1, 2 * D:3 * D].broadcast_to([P, D]),
                     [self.mrow_r[l]], [g1bc[j].r])
        lng = self.aalloc("lng", [D], F32, stream=True)
        lnb = self.aalloc("lnb", [D], F32, stream=True)
        self.dma("sp", lng, lng.ap, self.ln1_g[l:l + 1, :].broadcast_to([P, D]), [], [lng.r])
        self.dma("sp", lnb, lnb.ap, self.ln1_b[l:l + 1, :].broadcast_to([P, D]), [], [lnb.r])
        self.dma("sp", KT, KT.ap, self.KT_d.rearrange("h p t -> p h t"),
                 [r for rr in self.KT_r for r in rr], [KT.r])
        self.dma("sp", V, V.ap, self.VS_d.rearrange("(i p) c -> p i c", p=P), list(self.VS_r), [V.r])
        self.dma("pool", WO, WO.ap, self.w_out[l].rearrange("(k p) n -> p k n", p=P), [], [WO.r])
        Qz = [[self.aalloc("Qz%d_%d" % (m, i), [NH, 512], BF16, stream=True) for i in range(2)] for m in range(2)]
        for i in range(2):
            self.memset("pool", Qz[0][i].ap[64:128, :, :], 0.0, [Qz[0][i].r])
            self.memset("pool", Qz[1][i].ap[0:64, :, :], 0.0, [Qz[1][i].r])
        CVt = [self.aalloc("CVt%d" % i, [4, 512], BF16, stream=True) for i in range(2)]
        AT = [self.aalloc("AT%d" % i, [NH, 512], BF16) for i in range(2)]
        ET = [[self.aalloc("ET%d_%d" % (m, i), [512], BF16) for i in range(4)] for m in range(2)]
        EA1 = [self.aalloc("EA1_%d" % i, [512], F32) for i in range(2)]
        O12 = [self.aalloc("O12_%d" % m, [512], F32) for m in range(2)]
        S0c = self.aalloc("S0c", [512], F32)
        r12 = [self.aalloc("r%d" % m, [512], F32) for m in range(2)]
        a12 = [self.aalloc("a%d" % m, [512], F32) for m in range(2)]
        vv = [self.aalloc("vv%d" % i, [512], F32) for i in range(2)]
        sq = [self.aalloc("sq%d" % i, [512], F32) for i in range(2)]
        lnr = [self.aalloc("lnr%d" % i, [512], F32) for i in range(2)]
        xt = [self.aalloc("axt%d" % i, [D], F32, stream=True) for i in range(2)]
        z = [self.aalloc("az%d" % i, [D], F32) for i in range(2)]
        zn = z
        oo = [self.aalloc("ao%d" % i, [D], F32, stream=True) for i in range(2)]
        PB = self.PB
        ecnt = [0, 0]
        hcnt = 0
        ocnt = 0
        scnt = 0
        nqt = 8 if last else 9

        def load_q(qt):
            n = 256 if qt == 8 else 512
            tok0 = qt * 512
            bq = qt % 2
            for m in (0, 1):
                rows = slice(64 * m, 64 * m + 64)
                self.dma("sp", Qz[m][bq], Qz[m][bq].ap[rows, :, 0:n],
                         self.QT_d.rearrange("h p t -> p h t")[rows, :, tok0:tok0 + n],
                         [self.QT_r[h][qt] for h in range(NH)], [Qz[m][bq].r])
            pj = 2 if qt == 8 else (0 if qt < 4 else 1)
            self.dma("sp", CVt[bq], CVt[bq].ap[:, :, 0:n], self.CV_d.rearrange("c p t -> p c t")[:, :, tok0:tok0 + n],
                     [self.CV_r[c][pj] for c in range(4)], [CVt[bq].r])

        load_q(0)
        for qt in range(nqt):
            ctxq = qt == 8
            tok0 = qt * 512
            n = 256 if ctxq else 512
            chunks = [32, 33] if ctxq else list(range(NT))
            jmod = 2 if ctxq else b
            bq = qt % 2
            if qt + 1 < nqt:
                load_q(qt + 1)
            CV, A = CVt[bq], AT[bq]
            pending = None
            SCB = [[PB[0], PB[1]], [PB[2], PB[3]]]
            PVB = [PB[4], PB[5]]
            SM0 = PB[6]
            MISC = PB[7]
            nch = len(chunks)
            steps = [chunks[i:i + 2] for i in range(0, nch, 2)]
            nsteps = len(steps)

            def emit_scores(h, hb, si, ets):
                for j, kc in enumerate(steps[si]):
                    for m in (0, 1):
                        q = Qz[m][bq]
                        self.mm(SCB[m][j].ap[:, 0:n], KT.ap[:, h, kc * P:(kc + 1) * P], q.ap[:, h, 0:n], True, True,
                                [KT.r, q.r], [SCB[m][j].r])
                for j, kc in enumerate(steps[si]):
                    ci = 2 * si + j
                    for m in (0, 1):
                        et = ET[m][ecnt[m] % 4]
                        ecnt[m] += 1
                        self.act(et.ap[:, 0:n], SCB[m][j].ap[:, 0:n], AF.Exp, [SCB[m][j].r], [et.r], scale=QSCALE)
                        if m == 1:
                            ea = EA1[hb]
                            if ci == 0:
                                self.cp("dve", ea.ap[:, 0:n], et.ap[:, 0:n], [et.r], [ea.r])
                            else:
                                self.tt("dve", ea.ap[:, 0:n], ea.ap[:, 0:n], et.ap[:, 0:n], ALU.add, [ea.r, et.r], [ea.r])
                        ets[(si, j, m)] = et

            def epi_a(h, hb):
                self.S.op("dve", (lambda e, n=n: e.reciprocal(out=r12[0].ap[:, 0:n], in_=S0c.ap[:, 0:n])),
                          reads=[S0c.r], writes=[r12[0].r])
                self.tt("dve", a12[0].ap[:, 0:n], O12[0].ap[:, 0:n], r12[0].ap[:, 0:n], ALU.mult,
                        [O12[0].r, r12[0].r], [a12[0].r])

            def epi_b(h, hb):
                self.mm(MISC.ap[:, 0:n], self.onesf.ap, EA1[hb].ap[:, 0:n], True, True,
                        [self.onesf.r, EA1[hb].r], [MISC.r])
                self.S.op("dve", (lambda e, n=n: e.reciprocal(out=r12[1].ap[:, 0:n], in_=MISC.ap[:, 0:n])),
                          reads=[MISC.r], writes=[r12[1].r])
                self.tt("dve", a12[1].ap[:, 0:n], O12[1].ap[:, 0:n], r12[1].ap[:, 0:n], ALU.mult,
                        [O12[1].r, r12[1].r], [a12[1].r])
                self.stt("dve", vv[hb].ap[:, 0:n], a12[1].ap[:, 0:n], self.nlam.ap[:, l:l + 1], a12[0].ap[:, 0:n],
                         ALU.mult, ALU.add, [a12[0].r, a12[1].r, self.nlam.r], [vv[hb].r])
                self.tt("pool", sq[hb].ap[:, 0:n], vv[hb].ap[:, 0:n], vv[hb].ap[:, 0:n], ALU.mult, [vv[hb].r], [sq[hb].r])

            def tail(h, hb):
                self.mm(MISC.ap[:, 0:n], self.onesf.ap, sq[hb].ap[:, 0:n], True, True, [self.onesf.r, sq[hb].r], [MISC.r])
                self.act(lnr[hb].ap[:, 0:n], MISC.ap[:, 0:n], AF.Ln, [MISC.r], [lnr[hb].r], scale=1.0 / P, bias=HEAD_EPS)
                self.act(lnr[hb].ap[:, 0:n], lnr[hb].ap[:, 0:n], AF.Exp, [lnr[hb].r], [lnr[hb].r], scale=-0.5)
                self.stt("dve", A.ap[:, h, 0:n], vv[hb].ap[:, 0:n], self.gsc.ap[:, l, h:h + 1], lnr[hb].ap[:, 0:n],
                         ALU.mult, ALU.mult, [vv[hb].r, self.gsc.r, lnr[hb].r], [A.r])

            def flush(pend):
                stage = pend[2]
                if stage <= 0:
                    epi_a(pend[0], pend[1])
                if stage <= 1:
                    epi_b(pend[0], pend[1])
                tail(pend[0], pend[1])

            for h in range(NH):
                hb = hcnt % 2
                hcnt += 1
                if pending is not None and nsteps < 8:
                    flush(pending)
                    pending = None
                ets = {}
                emit_scores(h, hb, 0, ets)
                for si in range(nsteps):
                    if si + 1 < nsteps:
                        emit_scores(h, hb, si + 1, ets)
                    for m in (0, 1):
                        for j, kc in enumerate(steps[si]):
                            ci = 2 * si + j
                            self.mm(PVB[m].ap[:, 0:n], V.ap[:, kc, h * P:(h + 1) * P], ets[(si, j, m)].ap[:, 0:n], ci == 0,
                                    ci == nch - 1, [V.r, ets[(si, j, m)].r], [PVB[m].r])
                    for j, kc in enumerate(steps[si]):
                        ci = 2 * si + j
                        self.mm(SM0.ap[:, 0:n], self.onesb.ap, ets[(si, j, 0)].ap[:, 0:n], ci == 0, ci == nch - 1,
                                [self.onesb.r, ets[(si, j, 0)].r], [SM0.r])
                    if pending is not None:
                        if si == 1 and pending[2] == 0:
                            epi_a(pending[0], pending[1])
                            pending = (pending[0], pending[1], 1)
                        elif si == 3 and pending[2] == 1:
                            epi_b(pending[0], pending[1])
                            pending = (pending[0], pending[1], 2)
                        elif si == 6 and pending[2] == 2:
                            tail(pending[0], pending[1])
                            pending = None
                for m in (0, 1):
                    self.cp("dve", O12[m].ap[:, 0:n], PVB[m].ap[:, 0:n], [PVB[m].r], [O12[m].r])
                self.cp("dve", S0c.ap[:, 0:n], SM0.ap[:, 0:n], [SM0.r], [S0c.r])
                pending = (h, hb, 0)
            flush(pending)
            pending = None
            if self.stop_after == "att_q0":
                for nm, bf, nf in (("A", A, 2048), ("vv", vv[1], 512), ("lnr", lnr[1], 512), ("sq", sq[1], 512),
                                   ("a0", a12[0], 512), ("a1", a12[1], 512), ("r0", r12[0], 512), ("r1", r12[1], 512)):
                    if nm in self.dbg_out:
                        self.dbg_dump_bf16(nm, bf, nf)
                return
            for s in range(n // P):
                i = tok0 // P + s
                zi = scnt % 2
                scnt += 1
                src, src_r = self.xsrc(first, b, i)
                self.dma("sp", xt[zi], xt[zi].ap, src, [src_r] if src_r is not None else [], [xt[zi].r])
                for half in (0, 1):
                    bank = PB[6 + (ocnt % 2)]
                    ocnt += 1
                    for k in range(KD):
                        lhsT = A.ap[:, k, s * P:(s + 1) * P] if k < 4 else CV.ap[:, k - 4, s * P:(s + 1) * P]
                        self.mm(bank.ap, lhsT, WO.ap[:, k, half * 512:(half + 1) * 512], k == 0, k == KD - 1,
                                [A.r if k < 4 else CV.r, WO.r], [bank.r])
                    self.tt("dve", z[zi].ap[:, half * 512:(half + 1) * 512], bank.ap,
                            g1bc[jmod].ap[:, half * 512:(half + 1) * 512], ALU.mult, [bank.r, g1bc[jmod].r], [z[zi].r])
                self.stt("dve", z[zi].ap, xt[zi].ap, ALPHA, z[zi].ap, ALU.mult, ALU.add, [xt[zi].r, z[zi].r], [z[zi].r])
                self.post_ln(z[zi], self.stat[zi], zn[zi], oo[zi], lng, lnb, self.xs_d[b, i * P:(i + 1) * P, :],
                             self.xs_r[b][i])
            if self.stop_after == "att_q0b" or (self.stop_after == "att_q1b" and qt == 1):
                self.dbg_dump_bf16("A", AT[0], 2048)
                return

    def moe_phase(self, l, b, last):
        nsub = 32 if last else NT
        sizes = [(nsub + 2 - k) // 3 for k in range(3)]
        bounds = []
        s0 = 0
        for sz in sizes:
            bounds.append((s0, s0 + sz))
            s0 += sz
        for (i0, i1) in bounds:
            self.moe_pass(l, b, last, i0, i1)

    def moe_pass(self, l, b, last, i0, i1):
        self.phase_begin()
        PB = self.PB
        ns = i1 - i0
        ntok = ns * P
        HT = self.aalloc("HT", [KD, 12 * P], BF16)
        ACC = self.aalloc("ACC", [12, D], F32)
        acc_r = [Res("acc%d" % i) for i in range(ns)]
        gates = self.aalloc("gates", [12, NE], F32)
        WG = [self.aalloc("WG%d" % i, [KD, DEXP], BF16, stream=True) for i in range(2)]
        WU = [self.aalloc("WU%d" % i, [KD, DEXP], BF16, stream=True) for i in range(2)]
        WD = [self.aalloc("WD%d" % i, [4, D], BF16, stream=True) for i in range(2)]
        HE = [self.aalloc("HE%d" % i, [4, 512], BF16) for i in range(2)]
        SG = [self.aalloc("SG%d" % i, [512], F32) for i in range(2)]
        js = sorted(set((b if i < 32 else 2) for i in range(i0, i1)))
        g2bc = {}
        for j in js:
            g2bc[j] = self.aalloc("g2bc%d" % j, [D], F32, stream=True)
            self.dma("sp", g2bc[j], g2bc[j].ap, self.mrow_d[l, j:j + 1, 5 * D:6 * D].broadcast_to([P, D]),
                     [self.mrow_r[l]], [g2bc[j].r])
        lng = self.aalloc("lng2", [D], F32, stream=True)
        lnb = self.aalloc("lnb2", [D], F32, stream=True)
        self.dma("sp", lng, lng.ap, self.ln2_g[l:l + 1, :].broadcast_to([P, D]), [], [lng.r])
        self.dma("sp", lnb, lnb.ap, self.ln2_b[l:l + 1, :].broadcast_to([P, D]), [], [lnb.r])
        self.lnt_setup()
        z = [self.aalloc("mz%d" % i, [D], F32) for i in range(3)]
        zn = z
        oo = [self.aalloc("mo%d" % i, [D], F32, stream=True) for i in range(3)]

        def load_expert(e):
            bi = e % 2
            self.dma("pool", WG[bi], WG[bi].ap, self.w_gate[l, e].rearrange("(k p) n -> p k n", p=P), [], [WG[bi].r])
            self.dma("pool", WU[bi], WU[bi].ap, self.w_up[l, e].rearrange("(k p) n -> p k n", p=P), [], [WU[bi].r])
            self.dma("pool", WD[bi], WD[bi].ap, self.w_down[l, e].rearrange("(k p) n -> p k n", p=P), [], [WD[bi].r])

        load_expert(0)
        for ii in range(ns):
            i = i0 + ii
            j = b if i < 32 else 2
            self.lnt_tile(self.xs_d[b, i * P:(i + 1) * P, :], self.xs_r[b][i], HT, ii * P,
                          self.m1p.ap[:, l, 1, :, j], self.mT.ap[:, l, 24:32, j])
            pl = PB[6]
            for k in range(KD):
                self.mm(pl.ap[:, 0:NE], HT.ap[:, k, ii * P:(ii + 1) * P], self.rwb.ap[:, k, :], k == 0, k == KD - 1,
                        [HT.r, self.rwb.r], [pl.r])
            self.router(pl, gates, ii)
        tiles = []
        t0 = 0
        while t0 < ntok:
            n = min(512, ntok - t0)
            tiles.append((t0, n))
            t0 += n
        items = [(e, t0, n) for e in range(NE) for (t0, n) in tiles]
        ycnt = [0]

        def gu(idx):
            e, t0, n = items[idx]
            bi = e % 2
            he = HE[idx % 2]
            for c in range(4):
                pg, pu = PB[2 * (c % 2)], PB[2 * (c % 2) + 1]
                for k in range(KD):
                    self.mm(pg.ap[:, 0:n], WG[bi].ap[:, k, c * P:(c + 1) * P], HT.ap[:, k, t0:t0 + n], k == 0, k == KD - 1,
                            [WG[bi].r, HT.r], [pg.r])
                for k in range(KD):
                    self.mm(pu.ap[:, 0:n], WU[bi].ap[:, k, c * P:(c + 1) * P], HT.ap[:, k, t0:t0 + n], k == 0, k == KD - 1,
                            [WU[bi].r, HT.r], [pu.r])
                sg = SG[c % 2]
                self.act(sg.ap[:, 0:n], pg.ap[:, 0:n], AF.Silu, [pg.r], [sg.r])
                self.tt("dve", he.ap[:, c, 0:n], sg.ap[:, 0:n], pu.ap[:, 0:n], ALU.mult, [sg.r, pu.r], [he.r])

        def down(idx):
            e, t0, n = items[idx]
            bi = e % 2
            he = HE[idx % 2]
            for s in range(n // P):
                ii = t0 // P + s
                for half in (0, 1):
                    py = PB[4 + (ycnt[0] % 2)]
                    ycnt[0] += 1
                    for c in range(4):
                        self.mm(py.ap, he.ap[:, c, s * P:(s + 1) * P], WD[bi].ap[:, c, half * 512:(half + 1) * 512],
                                c == 0, c == 3, [he.r, WD[bi].r], [py.r])
                    dst = ACC.ap[:, ii, half * 512:(half + 1) * 512]
                    g = gates.ap[:, ii, e:e + 1]
                    if e == 0:
                        self.ts("dve", dst, py.ap, g, None, ALU.mult, None, [py.r, gates.r], [acc_r[ii]])
                    else:
                        self.stt("dve", dst, py.ap, g, dst, ALU.mult, ALU.add, [py.r, gates.r, acc_r[ii]], [acc_r[ii]])

        for idx in range(len(items)):
            e, t0, n = items[idx]
            if t0 == 0 and e + 1 < NE:
                load_expert(e + 1)
            if idx == 0:
                gu(0)
            if idx + 1 < len(items):
                gu(idx + 1)
            down(idx)
        for ii in range(ns):
            i = i0 + ii
            j = b if i < 32 else 2
            zi = ii % 3
            xt = self.lnt_xt[zi]
            self.dma("sp", xt, xt.ap, self.xs_d[b, i * P:(i + 1) * P, :], [self.xs_r[b][i]], [xt.r])
            self.tt("pool", z[zi].ap, ACC.ap[:, ii, :], g2bc[j].ap, ALU.mult, [acc_r[ii], g2bc[j].r], [z[zi].r])
            self.stt("dve", z[zi].ap, xt.ap, ALPHA, z[zi].ap, ALU.mult, ALU.add, [xt.r, z[zi].r], [z[zi].r])
            if last:
                dst, dst_r = self.y[b, i * P:(i + 1) * P, :], self.y_r
            else:
                dst, dst_r = self.xs_d[b, i * P:(i + 1) * P, :], self.xs_r[b][i]
            self.post_ln(z[zi], self.stat[zi], zn[zi], oo[zi], lng, lnb, dst, dst_r)

    def router(self, pl, gates, ii):
        t = self.rt[ii % 2]
        R = [t.r]
        a = t.ap
        sc, sel, m1, sel2, m2, gs, gm, ing, msk, oh, w = (a[:, 16 * i:16 * (i + 1)] for i in range(11))
        den = a[:, 176:177]
        self.act(sc, pl.ap[:, 0:NE], AF.Exp, [pl.r], R, scale=-1.0)
        self.ts("dve", sc, sc, 1.0, None, ALU.add, None, R, R)
        self.S.op("dve", lambda e: e.reciprocal(out=sc, in_=sc), reads=R, writes=R) if False else \
            self.S.op("dve", lambda e: e.reciprocal(out=sc, in_=sc), reads=R, writes=R)
        self.tt("dve", sel, sc, self.rbias.ap, ALU.add, R + [self.rbias.r], R)
        g4 = lambda v: v.rearrange("p (g e) -> p g e", g=4)
        self.red("dve", m1[:, 0:4], g4(sel), ALU.max, R, R)
        self.tt("dve", g4(sel2), g4(sel), m1[:, 0:4].unsqueeze(2).broadcast_to([P, 4, 4]), ALU.is_equal, R, R)
        self.stt("dve", sel2, sel2, -BIG, sel, ALU.mult, ALU.add, R, R)
        self.red("dve", m2[:, 0:4], g4(sel2), ALU.max, R, R)
        self.tt("dve", gs[:, 0:4], m1[:, 0:4], m2[:, 0:4], ALU.add, R, R)
        self.red("dve", gm[:, 0:1], gs[:, 0:4], ALU.max, R, R)
        self.ts("dve", ing[:, 0:4], gs[:, 0:4], gm[:, 0:1], None, ALU.is_equal, None, R, R)
        self.ts("dve", ing[:, 0:4], ing[:, 0:4], -1.0, BIG, ALU.add, ALU.mult, R, R)
        self.tt("dve", g4(msk), g4(sel), ing[:, 0:4].unsqueeze(2).broadcast_to([P, 4, 4]), ALU.add, R, R)
        self.red("dve", gm[:, 1:2], msk, ALU.max, R, R)
        self.ts("dve", oh, msk, gm[:, 1:2], None, ALU.is_equal, None, R, R)
        self.stt("dve", msk, oh, -BIG, msk, ALU.mult, ALU.add, R, R)
        self.red("dve", gm[:, 2:3], msk, ALU.max, R, R)
        self.ts("dve", sel2, msk, gm[:, 2:3], None, ALU.is_equal, None, R, R)
        self.tt("dve", oh, oh, sel2, ALU.add, R, R)
        self.tt("dve", w, sc, oh, ALU.mult, R, R)
        self.red("dve", den, w, ALU.add, R, R)
        self.S.op("dve", lambda e: e.reciprocal(out=den, in_=den), reads=R, writes=R)
        self.ts("dve", gates.ap[:, ii, :], w, den, None, ALU.mult, None, R, [gates.r])

    debug_layer = 0

    def dbg_dump_bf16(self, name, buf, nfree):
        self.S.barrier()
        out = self.dbg_out[name]
        tmp = self.aalloc("dbgtmp_" + name, [512], F32)
        flat = buf.ap
        if len(flat.shape) == 3:
            flat = flat.rearrange("p a b -> p (a b)")
        for o in range(0, nfree, 512):
            n = min(512, nfree - o)
            self.cp("dve", tmp.ap[:, 0:n], flat[:, o:o + n], [buf.r], [tmp.r])
            self.dma("sp", self.dbg_stream, out[:, o:o + n], tmp.ap[:, 0:n], [tmp.r], [self.dbg_r])
        self.S.barrier()


def _rope_tables():
    t = np.arange(NLAT, dtype=np.int32)
    row = (t // 64).astype(np.float32)
    col = (t % 64).astype(np.float32)
    inv = (np.float32(1.0) / (np.float32(10000.0) ** (np.arange(16, dtype=np.float32) / np.float32(16)))).astype(np.float32)
    cosT = np.zeros((P, NLAT), np.float32)
    sinT = np.zeros((P, NLAT), np.float32)
    for p in range(P):
        f = p % 64
        axis, half, fr = f // 32, (f // 16) % 2, f % 16
        pos = row if axis == 0 else col
        ang = (pos * inv[fr]).astype(np.float32)
        cosT[p] = np.cos(ang)
        sinT[p] = np.sin(ang) * (-1.0 if half == 0 else 1.0)
    return cosT, sinT


def _host_inputs(inputs):
    f = lambda k: np.ascontiguousarray(np.asarray(inputs[k], dtype=np.float32))
    w_in = f("w_in")
    cols = np.arange(512)
    perm = cols ^ 16
    q, k_ = w_in[:, :, 0:512], w_in[:, :, 512:1024]
    w_inx = np.ascontiguousarray(np.concatenate(
        [q, q[:, :, perm], k_, k_[:, :, perm], w_in[:, :, 1024:1536], w_in[:, :, 1536:2048], w_in[:, :, 2048:2560],
         w_in[:, :, 2560:3072]], axis=2))
    cosT, sinT = _rope_tables()
    shared = {
        "w_mod": f("w_mod"), "b_mod": f("b_mod"), "w_inx": w_inx,
        "dl": np.ascontiguousarray(f("diff_lambda").reshape(1, -1)),
        "gT": np.ascontiguousarray(f("attn_norm_g").reshape(DEPTH, NH, P).transpose(2, 0, 1)),
        "cwT": np.ascontiguousarray(f("conv_w").reshape(DEPTH, 3, 4, P).transpose(3, 0, 1, 2)),
        "cbT": np.ascontiguousarray(f("conv_b").reshape(DEPTH, 4, P).transpose(2, 0, 1)),
        "w_out": f("w_out"), "ln1_g": f("ln1_g"), "ln1_b": f("ln1_b"), "ln2_g": f("ln2_g"), "ln2_b": f("ln2_b"),
        "router_w": f("router_w"), "router_bias": np.ascontiguousarray(f("router_bias").reshape(1, NE)),
        "w_gate": f("w_gate"), "w_up": f("w_up"), "w_down": f("w_down"),
        "ident": np.eye(P, dtype=np.float32), "cosT": cosT, "sinT": sinT,
    }
    x, c, ctx, c_ctx = f("x"), f("c"), f("ctx"), f("c_ctx")
    in_maps = []
    for i in range(NCORES):
        cc = np.stack([c[2 * i], c[2 * i + 1], c_ctx], axis=0)
        ccT = np.ascontiguousarray(cc.reshape(3, KD, P).transpose(2, 1, 0))
        m = dict(shared)
        m["x"] = np.ascontiguousarray(x[2 * i:2 * i + 2])
        m["ctx"] = np.ascontiguousarray(ctx[2 * i:2 * i + 2])
        m["ccT"] = ccT
        in_maps.append(m)
    return in_maps


_NC_CACHE = {}


def kernel(**inputs):
    in_maps = _host_inputs(inputs)
    if "nc" not in _NC_CACHE:
        _NC_CACHE["nc"] = Builder().build()
    res = run_bass_kernel_spmd(_NC_CACHE["nc"], in_maps, core_ids=list(range(NCORES)))
    return np.concatenate([np.asarray(r["y"], dtype=np.float32) for r in res.results], axis=0)
```

```python
import math
from contextlib import ExitStack

import numpy as np

import concourse.bass as bass
import concourse.mybir as mybir
from concourse.bass_utils import run_bass_kernel_spmd

F32 = mybir.dt.float32
BF16 = mybir.dt.bfloat16
AF = mybir.ActivationFunctionType
ALU = mybir.AluOpType

P = 128
D = 1024
KD = 8
NLAT = 4096
NCTX = 256
NTOK = NLAT + NCTX
NT = NTOK // P
NH = 4
NE = 16
DEXP = 512
DEPTH = 2
ALPHA = (2 * DEPTH) ** 0.25
LN_EPS = 1e-6
HEAD_EPS = 1e-5
QSCALE = 64 ** -0.5
NCORES = 8
BIG = 100.0


class Res:
    __slots__ = ("name", "w", "r", "excl")

    def __init__(self, name, excl=False):
        self.name = name
        self.w = None
        self.r = {}
        self.excl = excl


class Stream:
    __slots__ = ("sem", "count")

    def __init__(self, sem):
        self.sem = sem
        self.count = 0


class EngQ:
    def __init__(self, name, sem, inorder):
        self.name = name
        self.sem = sem
        self.count = 0
        self.waited = {}
        self.ops = []
        self.inorder = inorder


class Sched:
    def __init__(self, nc, es):
        self.nc = nc
        self.es = es
        self.q = {}
        for name, inorder in (("pe", True), ("act", False), ("dve", False), ("pool", False), ("sp", True)):
            sem = es.enter_context(nc.semaphore("q_" + name))
            self.q[name] = EngQ(name, sem, inorder)
        self.streams = []

    def stream(self):
        s = Stream(self.es.enter_context(self.nc.semaphore("d%d" % len(self.streams))))
        self.streams.append(s)
        return s

    def _emit(self, eng, fn, reads, writes, stream):
        E = self.q[eng]
        waits = {}

        def need(sv):
            if sv is None:
                return
            s, v = sv
            if s is E.sem and E.inorder and stream is None:
                return
            k = id(s)
            if E.waited.get(k, 0) >= v:
                return
            if k not in waits or waits[k][1] < v:
                waits[k] = (s, v)

        for r in reads:
            need(r.w)
            if r.excl:
                for sv in r.r.values():
                    if sv[0] is not E.sem:
                        need(sv)
        for w in writes:
            need(w.w)
            for sv in w.r.values():
                need(sv)
        if stream is not None and stream.count:
            need((stream.sem, stream.count))
        for k, (s, v) in waits.items():
            E.waited[k] = v
        if stream is None:
            E.count += 1
            done = (E.sem, E.count)
            inc = 1
        else:
            stream.count += 16
            done = (stream.sem, stream.count)
            inc = 16
        E.ops.append((list(waits.values()), fn, done[0], inc))
        k = id(done[0])
        for r in reads:
            r.r[k] = done
        for w in writes:
            w.w = done
            w.r = {}
        return done

    def op(self, eng, fn, reads=(), writes=()):
        return self._emit(eng, fn, reads, writes, None)

    def dma(self, eng, stream, out, in_, reads=(), writes=()):
        return self._emit(eng, lambda e: e.dma_start(out=out, in_=in_), reads, writes, stream)

    def barrier(self, engines=("pe", "act", "dve", "pool", "sp")):
        targets = [(E.sem, E.count) for E in self.q.values() if E.count]
        targets += [(s.sem, s.count) for s in self.streams if s.count]
        for name in engines:
            E = self.q[name]
            waits = []
            for s, v in targets:
                if s is E.sem and E.inorder:
                    continue
                k = id(s)
                if E.waited.get(k, 0) >= v:
                    continue
                E.waited[k] = v
                waits.append((s, v))
            if waits:
                E.ops.append((waits, None, None, 0))

    def replay(self, block):
        def run(E):
            def body(eng):
                for waits, fn, sem, inc in E.ops:
                    for s, v in waits:
                        eng.wait_ge(s, v)
                    if fn is not None:
                        fn(eng).then_inc(sem, inc)
            return body

        block.tensor(run(self.q["pe"]))
        block.scalar(run(self.q["act"]))
        block.vector(run(self.q["dve"]))
        block.gpsimd(run(self.q["pool"]))
        block.sync(run(self.q["sp"]))


class Buf:
    __slots__ = ("ap", "r", "st")

    def __init__(self, ap, r, st=None):
        self.ap = ap
        self.r = r
        self.st = st


class Builder:
    def __init__(self, layers=(0, 1), debug=None, nbatch=2, stop_after=None, scratch_out=False, nb_run=None):
        self.layers = tuple(layers)
        self.debug = debug or {}
        self.nbatch = nbatch
        self.stop_after = stop_after
        self.scratch_out = scratch_out
        self.nb_run = nb_run if nb_run is not None else nbatch

    def mm(self, out, lhsT, rhs, start, stop, R, W, skip=False):
        self.S.op("pe", lambda e: e.matmul(out, lhsT=lhsT, rhs=rhs, start=start, stop=stop,
                                            skip_group_check=skip), reads=R, writes=W)

    def tr(self, out, in_, ident, R, W):
        self.S.op("pe", lambda e: e.transpose(out, in_, ident), reads=R, writes=W)

    def act(self, out, in_, func, R, W, scale=1.0, bias=0.0):
        self.S.op("act", lambda e: e.activation(out=out, in_=in_, func=func, bias=bias, scale=scale),
                  reads=R, writes=W)

    def tt(self, eng, out, in0, in1, op, R, W):
        self.S.op(eng, lambda e: e.tensor_tensor(out=out, in0=in0, in1=in1, op=op), reads=R, writes=W)

    def ts(self, eng, out, in0, s1, s2, op0, op1, R, W):
        if s2 is None:
            self.S.op(eng, lambda e: e.tensor_scalar(out=out, in0=in0, scalar1=s1, scalar2=None, op0=op0),
                      reads=R, writes=W)
        else:
            self.S.op(eng, lambda e: e.tensor_scalar(out=out, in0=in0, scalar1=s1, scalar2=s2, op0=op0, op1=op1),
                      reads=R, writes=W)

    def stt(self, eng, out, in0, scalar, in1, op0, op1, R, W):
        self.S.op(eng, lambda e: e.scalar_tensor_tensor(out=out, in0=in0, scalar=scalar, in1=in1, op0=op0, op1=op1),
                  reads=R, writes=W)

    def cp(self, eng, out, in_, R, W):
        self.S.op(eng, lambda e: e.tensor_copy(out=out, in_=in_), reads=R, writes=W)

    def red(self, eng, out, in_, op, R, W):
        self.S.op(eng, lambda e: e.tensor_reduce(out=out, in_=in_, axis=mybir.AxisListType.X, op=op),
                  reads=R, writes=W)

    def memset(self, eng, ap, val, W):
        self.S.op(eng, lambda e: e.memset(ap, val), writes=W)

    def dma(self, eng, buf_or_stream, out, in_, R, W):
        st = buf_or_stream.st if isinstance(buf_or_stream, Buf) else buf_or_stream
        self.S.dma(eng, st, out, in_, reads=R, writes=W)

    def sb(self, name, shape, dtype, stream=False):
        t = self.es.enter_context(self.nc.sbuf_tensor("s_" + name, list(shape), dtype))
        return Buf(t[:], Res(name), self.S.stream() if stream else None)

    def arena_reset(self):
        self.aoff = 0

    def aalloc(self, name, free_shape, dtype, stream=False):
        n = 1
        for s in free_shape:
            n *= s
        nb16 = n * (2 if dtype == F32 else 1)
        nb16 = (nb16 + 15) // 16 * 16
        assert self.aoff + nb16 <= self.asize, (name, self.aoff, nb16, self.asize)
        v = self.arena[:, self.aoff:self.aoff + n * (2 if dtype == F32 else 1)]
        self.aoff += nb16
        if dtype == F32:
            v = v.bitcast(F32)
        if len(free_shape) == 2:
            v = v.rearrange("p (a b) -> p a b", a=free_shape[0])
        elif len(free_shape) == 3:
            v = v.rearrange("p (a b c) -> p a b c", a=free_shape[0], b=free_shape[1])
        st = None
        if stream:
            if self.spool_i >= len(self.spool):
                self.spool.append(self.S.stream())
            st = self.spool[self.spool_i]
            self.spool_i += 1
        return Buf(v, Res(name), st)

    def phase_begin(self):
        self.S.barrier()
        self.arena_reset()
        self.spool_i = 0

    def build(self):
        nc = bass.Bass("TRN2", target_bir_lowering=False)
        self.nc = nc
        NB = self.nbatch
        din = lambda name, shape, dt=F32: nc.dram_tensor(name, list(shape), dt, kind="ExternalInput").ap()
        self.x_in = din("x", [NB, NLAT, D])
        self.ctx_in = din("ctx", [NB, NCTX, D])
        self.ccT_in = din("ccT", [P, KD, 3])
        self.w_mod = din("w_mod", [DEPTH, D, 6 * D])
        self.b_mod = din("b_mod", [DEPTH, 6 * D])
        self.w_inx = din("w_inx", [DEPTH, D, 4096])
        self.dl_in = din("dl", [1, DEPTH * 4 * 64])
        self.gT_in = din("gT", [P, DEPTH, NH])
        self.cwT_in = din("cwT", [P, DEPTH, 3, 4])
        self.cbT_in = din("cbT", [P, DEPTH, 4])
        self.w_out = din("w_out", [DEPTH, D, D])
        self.ln1_g = din("ln1_g", [DEPTH, D])
        self.ln1_b = din("ln1_b", [DEPTH, D])
        self.ln2_g = din("ln2_g", [DEPTH, D])
        self.ln2_b = din("ln2_b", [DEPTH, D])
        self.router_w = din("router_w", [D, NE])
        self.router_bias = din("router_bias", [1, NE])
        self.w_gate = din("w_gate", [DEPTH, NE, D, DEXP])
        self.w_up = din("w_up", [DEPTH, NE, D, DEXP])
        self.w_down = din("w_down", [DEPTH, NE, DEXP, D])
        self.ident_in = din("ident", [P, P])
        self.cos_in = din("cosT", [P, NLAT])
        self.sin_in = din("sinT", [P, NLAT])
        self.y = nc.dram_tensor("y", [NB, NLAT, D], F32, kind="ExternalOutput").ap()
        self.dbg_out = {}
        for name, shape in self.debug.items():
            self.dbg_out[name] = nc.dram_tensor("dbg_" + name, list(shape), F32, kind="ExternalOutput").ap()
        sk = {"kind": "ExternalOutput"} if self.scratch_out else {}
        self.xs_d = nc.dram_tensor("xs_d", [NB, NTOK, D], F32, **sk).ap()
        self.QT_d = nc.dram_tensor("QT_d", [NH, P, NTOK], BF16, **sk).ap()
        self.KT_d = nc.dram_tensor("KT_d", [NH, P, NTOK], BF16, **sk).ap()
        self.VS_d = nc.dram_tensor("VS_d", [NTOK, 512], BF16, **sk).ap()
        self.CV_d = nc.dram_tensor("CV_d", [4, P, NTOK], BF16, **sk).ap()
        self.mrow_d = nc.dram_tensor("mrow_d", [DEPTH, 3, 6 * D], F32, **sk).ap()
        self.xs_r = [[Res("xs%d_%d" % (b, i)) for i in range(NT)] for b in range(NB)]
        self.QT_r = [[Res("qt%d_%d" % (h, j)) for j in range(9)] for h in range(NH)]
        self.KT_r = [[Res("kt%d_%d" % (h, j)) for j in range(9)] for h in range(NH)]
        self.VS_r = [Res("vs%d" % i) for i in range(NT)]
        self.CV_r = [[Res("cv%d_%d" % (c, j)) for j in range(3)] for c in range(4)]
        self.mrow_r = [Res("mrowd%d" % l) for l in range(DEPTH)]
        self.y_r = Res("y")
        self.dbg_r = Res("dbg")

        with ExitStack() as es:
            self.es = es
            self.S = Sched(nc, es)
            S = self.S
            self.PB = []
            for i in range(8):
                t = es.enter_context(nc.psum_tensor("pb%d" % i, [P, 512], F32))
                self.PB.append(Buf(t[:], Res("pb%d" % i, excl=True)))
            self.identf = self.sb("identf", [P, P], F32, stream=True)
            self.identb = self.sb("identb", [P, P], BF16)
            self.onesb = self.sb("onesb", [P, P], BF16)
            self.onesf = self.sb("onesf", [P, P], F32)
            self.scT = self.sb("scT", [P, KD, 3], F32, stream=True)
            self.mT = self.sb("mT", [P, DEPTH, 48, 3], F32)
            self.m1p = self.sb("m1p", [P, DEPTH, 2, KD, 3], F32)
            self.dl = self.sb("dl", [P, DEPTH * 4 * 64], F32, stream=True)
            self.lamt = self.sb("lamt", [P, 8], F32)
            self.nlam = self.sb("nlam", [P, DEPTH], F32)
            self.gT = self.sb("gTs", [P, DEPTH, NH], F32, stream=True)
            self.gsc = self.sb("gsc", [P, DEPTH, NH], F32)
            self.cw = self.sb("cw", [P, DEPTH, 3, 4], F32, stream=True)
            self.cb = self.sb("cbs", [P, DEPTH, 4], F32, stream=True)
            self.rwb = self.sb("rwb", [P, KD, NE], BF16, stream=True)
            self.rbias = self.sb("rbias", [P, NE], F32, stream=True)
            self.stat = [self.sb("stat%d" % i, [P, 16], F32) for i in range(3)]
            self.rt = [self.sb("rt%d" % i, [P, 16 * 12], F32) for i in range(2)]
            self.asize = 99 * 1024
            arena_t = es.enter_context(nc.sbuf_tensor("arena", [P, self.asize], BF16))
            self.arena = arena_t[:]
            self.spool = []
            self.spool_i = 0
            self.arena_reset()
            self.dbg_stream = S.stream()

            self.setup_phase()
            first = True
            stop = self.stop_after
            done = False
            for l in self.layers:
                last = l == DEPTH - 1
                self.mod_phase(l)
                if stop == "mod":
                    break
                for b in range(self.nb_run):
                    self.lnt_phase_and_inproj(l, b, first, last)
                    if stop in ("lnt", "inproj"):
                        done = True
                        break
                    self.att_phase(l, b, first, last)
                    if stop in ("att", "att_q0", "att_q0b", "att_q1b"):
                        done = True
                        break
                    self.moe_phase(l, b, last)
                if done:
                    break
                first = False
            S.barrier()
            with nc.Block() as block:
                S.replay(block)
        return nc

    def xsrc(self, first, b, i):
        if first:
            if i < 32:
                return self.x_in[b, i * P:(i + 1) * P, :], None
            return self.ctx_in[b, (i - 32) * P:(i - 31) * P, :], None
        return self.xs_d[b, i * P:(i + 1) * P, :], self.xs_r[b][i]

    def setup_phase(self):
        S = self.S
        self.dma("sp", self.identf, self.identf.ap, self.ident_in, [], [self.identf.r])
        self.cp("dve", self.identb.ap, self.identf.ap, [self.identf.r], [self.identb.r])
        self.memset("pool", self.onesb.ap, 1.0, [self.onesb.r])
        self.memset("pool", self.onesf.ap, 1.0, [self.onesf.r])
        self.dma("sp", self.scT, self.scT.ap, self.ccT_in, [], [self.scT.r])
        self.act(self.scT.ap, self.scT.ap, AF.Silu, [self.scT.r], [self.scT.r])
        self.dma("sp", self.dl, self.dl.ap, self.dl_in.broadcast_to([P, DEPTH * 4 * 64]), [], [self.dl.r])
        self.dma("sp", self.gT, self.gT.ap, self.gT_in, [], [self.gT.r])
        self.dma("sp", self.cw, self.cw.ap, self.cwT_in, [], [self.cw.r])
        self.dma("sp", self.cb, self.cb.ap, self.cbT_in, [], [self.cb.r])
        self.dma("pool", self.rwb, self.rwb.ap, self.router_w.rearrange("(k p) e -> p k e", p=P), [], [self.rwb.r])
        self.dma("sp", self.rbias, self.rbias.ap, self.router_bias.broadcast_to([P, NE]), [], [self.rbias.r])
        dl4 = self.dl.ap.rearrange("p (l j d) -> p l j d", l=DEPTH, j=4)
        lt = self.lamt
        for l in range(DEPTH):
            lam_init = 0.8 - 0.6 * math.exp(-0.3 * l)
            tmp = self.rt[0]
            for j in range(2):
                self.tt("dve", tmp.ap[:, 0:64], dl4[:, l, 2 * j, :], dl4[:, l, 2 * j + 1, :], ALU.mult,
                        [self.dl.r], [tmp.r])
                self.red("dve", lt.ap[:, 4 * l + j:4 * l + j + 1], tmp.ap[:, 0:64], ALU.add, [tmp.r], [lt.r])
            self.act(lt.ap[:, 4 * l:4 * l + 2], lt.ap[:, 4 * l:4 * l + 2], AF.Exp, [lt.r], [lt.r])
            self.tt("dve", lt.ap[:, 4 * l + 2:4 * l + 3], lt.ap[:, 4 * l + 1:4 * l + 2], lt.ap[:, 4 * l:4 * l + 1],
                    ALU.subtract, [lt.r], [lt.r])
            self.ts("dve", self.nlam.ap[:, l:l + 1], lt.ap[:, 4 * l + 2:4 * l + 3], -lam_init, None, ALU.add, None,
                    [lt.r], [self.nlam.r])
            self.ts("dve", self.gsc.ap[:, l, :], self.gT.ap[:, l, :], 1.0 - lam_init, None, ALU.mult, None,
                    [self.gT.r], [self.gsc.r])

    def mod_phase(self, l):
        self.phase_begin()
        wm = [self.aalloc("wm%d" % i, [KD, 512], F32, stream=True) for i in range(2)]
        mrow = self.aalloc("mrow", [6 * D], F32, stream=True)
        bm3 = self.aalloc("bm3", [6 * D], F32, stream=True)
        self.dma("sp", bm3, bm3.ap[0:3, :], self.b_mod[l:l + 1, :].broadcast_to([3, 6 * D]), [], [bm3.r])
        for cb in range(12):
            w = wm[cb % 2]
            self.dma("sp", w, w.ap, self.w_mod[l, :, cb * 512:(cb + 1) * 512].rearrange("(k p) n -> p k n", p=P),
                     [], [w.r])
            bank = self.PB[cb % 2]
            for k in range(KD):
                self.mm(bank.ap[0:3, :], self.scT.ap[:, k, :], w.ap[:, k, :], k == 0, k == KD - 1,
                        [self.scT.r, w.r], [bank.r])
            self.tt("dve", mrow.ap[0:3, cb * 512:(cb + 1) * 512], bank.ap[0:3, :], bm3.ap[0:3, cb * 512:(cb + 1) * 512],
                    ALU.add, [bank.r, bm3.r], [mrow.r])
        self.dma("sp", mrow, self.mrow_d[l], mrow.ap[0:3, :], [mrow.r], [self.mrow_r[l]])
        pT = self.PB[2]
        pTv = pT.ap[:, 0:144].rearrange("p (j c) -> p j c", c=3)
        for jn in range(48):
            self.tr(pTv[:, jn, :], mrow.ap[0:3, jn * P:(jn + 1) * P], self.identf.ap[0:3, 0:3],
                    [mrow.r, self.identf.r], [pT.r])
        self.cp("dve", self.mT.ap[:, l, :, :], pTv, [pT.r], [self.mT.r])
        for w_, v in ((0, 1), (1, 4)):
            self.ts("dve", self.m1p.ap[:, l, w_, :, :], self.mT.ap[:, l, v * 8:(v + 1) * 8, :], 1.0, None, ALU.add, None,
                    [self.mT.r], [self.m1p.r])

    def lnt_setup(self):
        self.lnt_xt = [self.aalloc("lxt%d" % i, [D], F32, stream=True) for i in range(3)]
        self.lnt_xn = [self.aalloc("lxn%d" % i, [D], BF16) for i in range(3)]
        self.lnt_cnt = 0

    def ln_stats(self, z, st):
        self.S.op("dve", lambda e: e.bn_stats(st.ap[:, 0:6], z.ap[:, 0:512]), reads=[z.r], writes=[st.r])
        self.S.op("dve", lambda e: e.bn_stats(st.ap[:, 6:12], z.ap[:, 512:1024]), reads=[z.r], writes=[st.r])
        self.S.op("dve", lambda e: e.bn_aggr(st.ap[:, 12:14], st.ap[:, 0:12]), reads=[st.r], writes=[st.r])
        self.act(st.ap[:, 14:15], st.ap[:, 13:14], AF.Ln, [st.r], [st.r], bias=LN_EPS)
        self.act(st.ap[:, 14:15], st.ap[:, 14:15], AF.Exp, [st.r], [st.r], scale=-0.5)
        self.stt("dve", st.ap[:, 15:16], st.ap[:, 12:13], -1.0, st.ap[:, 14:15], ALU.mult, ALU.mult, [st.r], [st.r])

    def lnt_tile(self, src_ap, src_r, UT, tok_off, scp, shv):
        c = self.lnt_cnt
        self.lnt_cnt += 1
        bi = c % 3
        xt, xn, st = self.lnt_xt[bi], self.lnt_xn[bi], self.stat[bi]
        self.dma("sp", xt, xt.ap, src_ap, [src_r] if src_r is not None else [], [xt.r])
        self.ln_stats(xt, st)
        self.ts("dve", xn.ap, xt.ap, st.ap[:, 12:13], st.ap[:, 14:15], ALU.subtract, ALU.mult, [xt.r, st.r], [xn.r])
        pa, pb = self.PB[2 * bi], self.PB[2 * bi + 1]
        for half, bank in ((0, pa), (1, pb)):
            pv = bank.ap.bitcast(BF16).rearrange("p (k t) -> p k t", k=8)
            for kk in range(4):
                k = half * 4 + kk
                self.tr(pv[:, kk, :], xn.ap[:, k * P:(k + 1) * P], self.identb.ap, [xn.r, self.identb.r], [bank.r])
            for kk in range(4):
                k = half * 4 + kk
                dst = UT.ap[:, k, tok_off:tok_off + P]
                if half == 0:
                    self.act(dst, pv[:, kk, :], AF.Identity, [bank.r, self.m1p.r, self.mT.r], [UT.r],
                             scale=scp[:, k:k + 1], bias=shv[:, k:k + 1])
                else:
                    self.ts("dve", dst, pv[:, kk, :], scp[:, k:k + 1], shv[:, k:k + 1], ALU.mult, ALU.add,
                            [bank.r, self.m1p.r, self.mT.r], [UT.r])

    def lnt_phase_and_inproj(self, l, b, first, last):
        self.phase_begin()
        UT = self.aalloc("UT", [KD, NTOK], BF16)
        self.lnt_setup()
        for i in range(NT):
            j = b if i < 32 else 2
            src, src_r = self.xsrc(first, b, i)
            self.lnt_tile(src, src_r, UT, i * P, self.m1p.ap[:, l, 0, :, j], self.mT.ap[:, l, 0:8, j])
        if "uT" in self.dbg_out and l == self.debug_layer and b == 0:
            self.dbg_dump_bf16("uT", UT, KD * NTOK)
        if self.stop_after == "lnt":
            return
        wA = self.aalloc("wA", [KD, 512], BF16, stream=True)
        wB = self.aalloc("wB", [KD, 512], BF16, stream=True)
        wC = self.aalloc("wC", [KD, 512], BF16, stream=True)
        mark = self.aoff
        cosT = self.aalloc("cosT", [NLAT], F32, stream=True)
        sinT = self.aalloc("sinT", [NLAT], F32, stream=True)
        t1 = [self.aalloc("rt1_%d" % i, [512], F32) for i in range(2)]
        t2 = [self.aalloc("rt2_%d" % i, [512], F32) for i in range(2)]
        qo = [self.aalloc("qo%d" % i, [512], BF16, stream=True) for i in range(2)]
        self.dma("sp", cosT, cosT.ap, self.cos_in, [], [cosT.r])
        self.dma("sp", sinT, sinT.ap, self.sin_in, [], [sinT.r])
        wx = self.w_inx[l].rearrange("(k p) n -> p k n", p=P)
        cnt = 0
        for which, base, dst, dst_r in (("q", 0, self.QT_d, self.QT_r), ("k", 1024, self.KT_d, self.KT_r)):
            self.dma("pool", wA, wA.ap, wx[:, :, base:base + 512], [], [wA.r])
            self.dma("pool", wB, wB.ap, wx[:, :, base + 512:base + 1024], [], [wB.r])
            for h in range(NH):
                for jt in range(9):
                    ctx_tile = jt == 8
                    if ctx_tile and which == "q" and last:
                        continue
                    tok0 = jt * 512
                    n = 256 if ctx_tile else 512
                    bi = cnt % 2
                    cnt += 1
                    pa, pb = self.PB[4 + bi], self.PB[6 + bi]
                    for k in range(KD):
                        self.mm(pa.ap[:, 0:n], wA.ap[:, k, h * P:(h + 1) * P], UT.ap[:, k, tok0:tok0 + n],
                                k == 0, k == KD - 1, [wA.r, UT.r], [pa.r])
                    o = qo[bi]
                    if not ctx_tile:
                        for k in range(KD):
                            self.mm(pb.ap[:, 0:n], wB.ap[:, k, h * P:(h + 1) * P], UT.ap[:, k, tok0:tok0 + n],
                                    k == 0, k == KD - 1, [wB.r, UT.r], [pb.r])
                        self.tt("dve", t1[bi].ap, pa.ap, cosT.ap[:, tok0:tok0 + n], ALU.mult, [pa.r, cosT.r], [t1[bi].r])
                        self.tt("dve", t2[bi].ap, pb.ap, sinT.ap[:, tok0:tok0 + n], ALU.mult, [pb.r, sinT.r], [t2[bi].r])
                        self.tt("pool", o.ap, t1[bi].ap, t2[bi].ap, ALU.add, [t1[bi].r, t2[bi].r], [o.r])
                    else:
                        self.act(o.ap[:, 0:n], pa.ap[:, 0:n], AF.Copy, [pa.r], [o.r])
                    self.dma("sp", o, dst[h, :, tok0:tok0 + n], o.ap[:, 0:n], [o.r], [dst_r[h][jt]])
        self.dma("pool", wC, wC.ap, wx[:, :, 2048:2560], [], [wC.r])
        vo = qo
        for i in range(NT):
            bi = i % 2
            pv = self.PB[4 + bi]
            for k in range(KD):
                self.mm(pv.ap, UT.ap[:, k, i * P:(i + 1) * P], wC.ap[:, k, :], k == 0, k == KD - 1, [UT.r, wC.r], [pv.r])
            self.act(vo[bi].ap, pv.ap, AF.Copy, [pv.r], [vo[bi].r])
            self.dma("sp", vo[bi], self.VS_d[i * P:(i + 1) * P, :], vo[bi].ap, [vo[bi].r], [self.VS_r[i]])
        self.S.barrier()
        self.aoff = mark
        GL = NTOK + 8
        G = self.aalloc("G", [GL], F32)
        GB = self.aalloc("GB", [NTOK], F32)
        tmpc = [self.aalloc("tmpc%d" % i, [512], F32) for i in range(2)]
        T = [self.aalloc("T%d" % i, [2048], F32) for i in range(2)]
        Y = [self.aalloc("Y%d" % i, [2048], BF16, stream=True) for i in range(2)]
        self.dma("pool", wA, wA.ap, wx[:, :, 2560:3072], [], [wA.r])
        self.dma("pool", wB, wB.ap, wx[:, :, 3072:3584], [], [wB.r])
        self.dma("pool", wC, wC.ap, wx[:, :, 3584:4096], [], [wC.r])
        self.memset("pool", G.ap[:, 0:1], 0.0, [G.r])
        self.memset("pool", G.ap[:, 4097:4099], 0.0, [G.r])
        self.memset("pool", G.ap[:, 4355:4356], 0.0, [G.r])

        def gidx(t):
            return 1 + t if t < NLAT else 4099 + (t - NLAT)

        ntile = 8 if last else 9
        pieces = [(0, 2048, 0), (2048, 2048, 1)] + ([] if last else [(NLAT, NCTX, 2)])
        ycnt = 0
        for c in range(4):
            for jt in range(ntile):
                tok0 = jt * 512
                n = 256 if jt == 8 else 512
                bi = jt % 2
                pg = [self.PB[bi * 3 + x] for x in range(3)]
                for x, w in enumerate((wA, wB, wC)):
                    for k in range(KD):
                        self.mm(pg[x].ap[:, 0:n], w.ap[:, k, c * P:(c + 1) * P], UT.ap[:, k, tok0:tok0 + n],
                                k == 0, k == KD - 1, [w.r, UT.r], [pg[x].r])
                self.act(GB.ap[:, tok0:tok0 + n], pg[0].ap[:, 0:n], AF.Copy, [pg[0].r], [GB.r])
                self.act(tmpc[bi].ap[:, 0:n], pg[1].ap[:, 0:n], AF.Copy, [pg[1].r], [tmpc[bi].r])
                g0 = gidx(tok0)
                self.tt("dve", G.ap[:, g0:g0 + n], tmpc[bi].ap[:, 0:n], pg[2].ap[:, 0:n], ALU.mult,
                        [tmpc[bi].r, pg[2].r], [G.r])
            for (t0, n, pj) in pieces:
                bi = ycnt % 2
                ycnt += 1
                g0 = gidx(t0)
                Tb, Yb = T[bi], Y[bi]
                self.act(Tb.ap[:, 0:n], G.ap[:, g0:g0 + n], AF.Identity, [G.r, self.cw.r, self.cb.r], [Tb.r],
                         scale=self.cw.ap[:, l, 1, c:c + 1], bias=self.cb.ap[:, l, c:c + 1])
                self.stt("dve", Tb.ap[:, 0:n], G.ap[:, g0 - 1:g0 - 1 + n], self.cw.ap[:, l, 0, c:c + 1], Tb.ap[:, 0:n],
                         ALU.mult, ALU.add, [G.r, self.cw.r, Tb.r], [Tb.r])
                self.stt("dve", Tb.ap[:, 0:n], G.ap[:, g0 + 1:g0 + 1 + n], self.cw.ap[:, l, 2, c:c + 1], Tb.ap[:, 0:n],
                         ALU.mult, ALU.add, [G.r, self.cw.r, Tb.r], [Tb.r])
                self.tt("pool", Yb.ap[:, 0:n], GB.ap[:, t0:t0 + n], Tb.ap[:, 0:n], ALU.mult, [GB.r, Tb.r], [Yb.r])
                self.dma("sp", Yb, self.CV_d[c, :, t0:t0 + n], Yb.ap[:, 0:n], [Yb.r], [self.CV_r[c][pj]])

    def post_ln(self, z, st, zn, o, lng, lnb, dst_ap, dst_r):
        self.ln_stats(z, st)
        self.stt("dve", z.ap, z.ap, st.ap[:, 12:13], lng.ap, ALU.subtract, ALU.mult, [z.r, st.r, lng.r], [z.r])
        self.stt("dve", o.ap, z.ap, st.ap[:, 14:15], lnb.ap, ALU.mult, ALU.add, [z.r, st.r, lnb.r], [o.r])
        self.dma("sp", o, dst_ap, o.ap, [o.r], [dst_r])

    def att_phase(self, l, b, first, last):
        self.phase_begin()
        KT = self.aalloc("KT", [NH, NTOK], BF16, stream=True)
        V = self.aalloc("V", [NT, 512], BF16, stream=True)
        WO = self.aalloc("WO", [KD, D], BF16, stream=True)
        g1bc = {}
        for j in ([b] if last else [b, 2]):
            g1bc[j] = self.aalloc("g1bc%d" % j, [D], F32, stream=True)
            self.dma("sp", g1bc[j], g1bc[j].ap, self.mrow_d[l, j:j + 1, 2 * D:3 * D].broadcast_to([P, D]),
                     [self.mrow_r[l]], [g1bc[j].r])
        lng = self.aalloc("lng", [D], F32, stream=True)
        lnb = self.aalloc("lnb", [D], F32, stream=True)
        self.dma("sp", lng, lng.ap, self.ln1_g[l:l + 1, :].broadcast_to([P, D]), [], [lng.r])
        self.dma("sp", lnb, lnb.ap, self.ln1_b[l:l + 1, :].broadcast_to([P, D]), [], [lnb.r])
        self.dma("sp", KT, KT.ap, self.KT_d.rearrange("h p t -> p h t"),
                 [r for rr in self.KT_r for r in rr], [KT.r])
        self.dma("sp", V, V.ap, self.VS_d.rearrange("(i p) c -> p i c", p=P), list(self.VS_r), [V.r])
        self.dma("pool", WO, WO.ap, self.w_out[l].rearrange("(k p) n -> p k n", p=P), [], [WO.r])
        Qz = [[self.aalloc("Qz%d_%d" % (m, i), [NH, 512], BF16, stream=True) for i in range(2)] for m in range(2)]
        for i in range(2):
            self.memset("pool", Qz[0][i].ap[64:128, :, :], 0.0, [Qz[0][i].r])
            self.memset("pool", Qz[1][i].ap[0:64, :, :], 0.0, [Qz[1][i].r])
        CVt = [self.aalloc("CVt%d" % i, [4, 512], BF16, stream=True) for i in range(2)]
        AT = [self.aalloc("AT%d" % i, [NH, 512], BF16) for i in range(2)]
        ET = [[self.aalloc("ET%d_%d" % (m, i), [512], BF16) for i in range(4)] for m in range(2)]
        EA1 = [self.aalloc("EA1_%d" % i, [512], F32) for i in range(2)]
        O12 = [self.aalloc("O12_%d" % m, [512], F32) for m in range(2)]
        S0c = self.aalloc("S0c", [512], F32)
        r12 = [self.aalloc("r%d" % m, [512], F32) for m in range(2)]
        a12 = [self.aalloc("a%d" % m, [512], F32) for m in range(2)]
        vv = [self.aalloc("vv%d" % i, [512], F32) for i in range(2)]
        sq = [self.aalloc("sq%d" % i, [512], F32) for i in range(2)]
        lnr = [self.aalloc("lnr%d" % i, [512], F32) for i in range(2)]
        xt = [self.aalloc("axt%d" % i, [D], F32, stream=True) for i in range(2)]
        z = [self.aalloc("az%d" % i, [D], F32) for i in range(2)]
        zn = z
        oo = [self.aalloc("ao%d" % i, [D], F32, stream=True) for i in range(2)]
        PB = self.PB
        ecnt = [0, 0]
        hcnt = 0
        ocnt = 0
        scnt = 0
        nqt = 8 if last else 9

        def load_q(qt):
            n = 256 if qt == 8 else 512
            tok0 = qt * 512
            bq = qt % 2
            for m in (0, 1):
                rows = slice(64 * m, 64 * m + 64)
                self.dma("sp", Qz[m][bq], Qz[m][bq].ap[rows, :, 0:n],
                         self.QT_d.rearrange("h p t -> p h t")[rows, :, tok0:tok0 + n],
                         [self.QT_r[h][qt] for h in range(NH)], [Qz[m][bq].r])
            pj = 2 if qt == 8 else (0 if qt < 4 else 1)
            self.dma("sp", CVt[bq], CVt[bq].ap[:, :, 0:n], self.CV_d.rearrange("c p t -> p c t")[:, :, tok0:tok0 + n],
                     [self.CV_r[c][pj] for c in range(4)], [CVt[bq].r])

        load_q(0)
        for qt in range(nqt):
            ctxq = qt == 8
            tok0 = qt * 512
            n = 256 if ctxq else 512
            chunks = [32, 33] if ctxq else list(range(NT))
            jmod = 2 if ctxq else b
            bq = qt % 2
            if qt + 1 < nqt:
                load_q(qt + 1)
            CV, A = CVt[bq], AT[bq]
            pending = None
            SCB = [[PB[0], PB[1]], [PB[2], PB[3]]]
            PVB = [PB[4], PB[5]]
            SM0 = PB[6]
            MISC = PB[7]
            nch = len(chunks)
            steps = [chunks[i:i + 2] for i in range(0, nch, 2)]
            nsteps = len(steps)

            def emit_scores(h, hb, si, ets):
                for j, kc in enumerate(steps[si]):
                    for m in (0, 1):
                        q = Qz[m][bq]
                        self.mm(SCB[m][j].ap[:, 0:n], KT.ap[:, h, kc * P:(kc + 1) * P], q.ap[:, h, 0:n], True, True,
                                [KT.r, q.r], [SCB[m][j].r])
                for j, kc in enumerate(steps[si]):
                    ci = 2 * si + j
                    for m in (0, 1):
                        et = ET[m][ecnt[m] % 4]
                        ecnt[m] += 1
                        self.act(et.ap[:, 0:n], SCB[m][j].ap[:, 0:n], AF.Exp, [SCB[m][j].r], [et.r], scale=QSCALE)
                        if m == 1:
                            ea = EA1[hb]
                            if ci == 0:
                                self.cp("dve", ea.ap[:, 0:n], et.ap[:, 0:n], [et.r], [ea.r])
                            else:
                                self.tt("dve", ea.ap[:, 0:n], ea.ap[:, 0:n], et.ap[:, 0:n], ALU.add, [ea.r, et.r], [ea.r])
                        ets[(si, j, m)] = et

            def epi_a(h, hb):
                self.S.op("dve", (lambda e, n=n: e.reciprocal(out=r12[0].ap[:, 0:n], in_=S0c.ap[:, 0:n])),
                          reads=[S0c.r], writes=[r12[0].r])
                self.tt("dve", a12[0].ap[:, 0:n], O12[0].ap[:, 0:n], r12[0].ap[:, 0:n], ALU.mult,
                        [O12[0].r, r12[0].r], [a12[0].r])

            def epi_b(h, hb):
                self.mm(MISC.ap[:, 0:n], self.onesf.ap, EA1[hb].ap[:, 0:n], True, True,
                        [self.onesf.r, EA1[hb].r], [MISC.r])
                self.S.op("dve", (lambda e, n=n: e.reciprocal(out=r12[1].ap[:, 0:n], in_=MISC.ap[:, 0:n])),
                          reads=[MISC.r], writes=[r12[1].r])
                self.tt("dve", a12[1].ap[:, 0:n], O12[1].ap[:, 0:n], r12[1].ap[:, 0:n], ALU.mult,
                        [O12[1].r, r12[1].r], [a12[1].r])
                self.stt("dve", vv[hb].ap[:, 0:n], a12[1].ap[:, 0:n], self.nlam.ap[:, l:l + 1], a12[0].ap[:, 0:n],
                         ALU.mult, ALU.add, [a12[0].r, a12[1].r, self.nlam.r], [vv[hb].r])
                self.tt("pool", sq[hb].ap[:, 0:n], vv[hb].ap[:, 0:n], vv[hb].ap[:, 0:n], ALU.mult, [vv[hb].r], [sq[hb].r])

            def tail(h, hb):
                self.mm(MISC.ap[:, 0:n], self.onesf.ap, sq[hb].ap[:, 0:n], True, True, [self.onesf.r, sq[hb].r], [MISC.r])
                self.act(lnr[hb].ap[:, 0:n], MISC.ap[:, 0:n], AF.Ln, [MISC.r], [lnr[hb].r], scale=1.0 / P, bias=HEAD_EPS)
                self.act(lnr[hb].ap[:, 0:n], lnr[hb].ap[:, 0:n], AF.Exp, [lnr[hb].r], [lnr[hb].r], scale=-0.5)
                self.stt("dve", A.ap[:, h, 0:n], vv[hb].ap[:, 0:n], self.gsc.ap[:, l, h:h + 1], lnr[hb].ap[:, 0:n],
                         ALU.mult, ALU.mult, [vv[hb].r, self.gsc.r, lnr[hb].r], [A.r])

            def flush(pend):
                stage = pend[2]
                if stage <= 0:
                    epi_a(pend[0], pend[1])
                if stage <= 1:
                    epi_b(pend[0], pend[1])
                tail(pend[0], pend[1])

            for h in range(NH):
                hb = hcnt % 2
                hcnt += 1
                if pending is not None and nsteps < 8:
                    flush(pending)
                    pending = None
                ets = {}
                emit_scores(h, hb, 0, ets)
                for si in range(nsteps):
                    if si + 1 < nsteps:
                        emit_scores(h, hb, si + 1, ets)
                    for m in (0, 1):
                        for j, kc in enumerate(steps[si]):
                            ci = 2 * si + j
                            self.mm(PVB[m].ap[:, 0:n], V.ap[:, kc, h * P:(h + 1) * P], ets[(si, j, m)].ap[:, 0:n], ci == 0,
                                    ci == nch - 1, [V.r, ets[(si, j, m)].r], [PVB[m].r])
                    for j, kc in enumerate(steps[si]):
                        ci = 2 * si + j
                        self.mm(SM0.ap[:, 0:n], self.onesb.ap, ets[(si, j, 0)].ap[:, 0:n], ci == 0, ci == nch - 1,
                                [self.onesb.r, ets[(si, j, 0)].r], [SM0.r])
                    if pending is not None:
                        if si == 1 and pending[2] == 0:
                            epi_a(pending[0], pending[1])
                            pending = (pending[0], pending[1], 1)
                        elif si == 3 and pending[2] == 1:
                            epi_b(pending[0], pending[1])
                            pending = (pending[0], pending[1], 2)
                        elif si == 6 and pending[2] == 2:
                            tail(pending[0], pending[1])
                            pending = None
                for m in (0, 1):
                    self.cp("dve", O12[m].ap[:, 0:n], PVB[m].ap[:, 0:n], [PVB[m].r], [O12[m].r])
                self.cp("dve", S0c.ap[:, 0:n], SM0.ap[:, 0:n], [SM0.r], [S0c.r])
                pending = (h, hb, 0)
            flush(pending)
            pending = None
            if self.stop_after == "att_q0":
                for nm, bf, nf in (("A", A, 2048), ("vv", vv[1], 512), ("lnr", lnr[1], 512), ("sq", sq[1], 512),
                                   ("a0", a12[0], 512), ("a1", a12[1], 512), ("r0", r12[0], 512), ("r1", r12[1], 512)):
                    if nm in self.dbg_out:
                        self.dbg_dump_bf16(nm, bf, nf)
                return
            for s in range(n // P):
                i = tok0 // P + s
                zi = scnt % 2
                scnt += 1
                src, src_r = self.xsrc(first, b, i)
                self.dma("sp", xt[zi], xt[zi].ap, src, [src_r] if src_r is not None else [], [xt[zi].r])
                for half in (0, 1):
                    bank = PB[6 + (ocnt % 2)]
                    ocnt += 1
                    for k in range(KD):
                        lhsT = A.ap[:, k, s * P:(s + 1) * P] if k < 4 else CV.ap[:, k - 4, s * P:(s + 1) * P]
                        self.mm(bank.ap, lhsT, WO.ap[:, k, half * 512:(half + 1) * 512], k == 0, k == KD - 1,
                                [A.r if k < 4 else CV.r, WO.r], [bank.r])
                    self.tt("dve", z[zi].ap[:, half * 512:(half + 1) * 512], bank.ap,
                            g1bc[jmod].ap[:, half * 512:(half + 1) * 512], ALU.mult, [bank.r, g1bc[jmod].r], [z[zi].r])
                self.stt("dve", z[zi].ap, xt[zi].ap, ALPHA, z[zi].ap, ALU.mult, ALU.add, [xt[zi].r, z[zi].r], [z[zi].r])
                self.post_ln(z[zi], self.stat[zi], zn[zi], oo[zi], lng, lnb, self.xs_d[b, i * P:(i + 1) * P, :],
                             self.xs_r[b][i])
            if self.stop_after == "att_q0b" or (self.stop_after == "att_q1b" and qt == 1):
                self.dbg_dump_bf16("A", AT[0], 2048)
                return

    def moe_phase(self, l, b, last):
        nsub = 32 if last else NT
        sizes = [(nsub + 2 - k) // 3 for k in range(3)]
        bounds = []
        s0 = 0
        for sz in sizes:
            bounds.append((s0, s0 + sz))
            s0 += sz
        for (i0, i1) in bounds:
            self.moe_pass(l, b, last, i0, i1)

    def moe_pass(self, l, b, last, i0, i1):
        self.phase_begin()
        PB = self.PB
        ns = i1 - i0
        ntok = ns * P
        HT = self.aalloc("HT", [KD, 12 * P], BF16)
        ACC = self.aalloc("ACC", [12, D], F32)
        acc_r = [Res("acc%d" % i) for i in range(ns)]
        gates = self.aalloc("gates", [12, NE], F32)
        WG = [self.aalloc("WG%d" % i, [KD, DEXP], BF16, stream=True) for i in range(2)]
        WU = [self.aalloc("WU%d" % i, [KD, DEXP], BF16, stream=True) for i in range(2)]
        WD = [self.aalloc("WD%d" % i, [4, D], BF16, stream=True) for i in range(2)]
        HE = [self.aalloc("HE%d" % i, [4, 512], BF16) for i in range(2)]
        SG = [self.aalloc("SG%d" % i, [512], F32) for i in range(2)]
        js = sorted(set((b if i < 32 else 2) for i in range(i0, i1)))
        g2bc = {}
        for j in js:
            g2bc[j] = self.aalloc("g2bc%d" % j, [D], F32, stream=True)
            self.dma("sp", g2bc[j], g2bc[j].ap, self.mrow_d[l, j:j + 1, 5 * D:6 * D].broadcast_to([P, D]),
                     [self.mrow_r[l]], [g2bc[j].r])
        lng = self.aalloc("lng2", [D], F32, stream=True)
        lnb = self.aalloc("lnb2", [D], F32, stream=True)
        self.dma("sp", lng, lng.ap, self.ln2_g[l:l + 1, :].broadcast_to([P, D]), [], [lng.r])
        self.dma("sp", lnb, lnb.ap, self.ln2_b[l:l + 1, :].broadcast_to([P, D]), [], [lnb.r])
        self.lnt_setup()
        z = [self.aalloc("mz%d" % i, [D], F32) for i in range(3)]
        zn = z
        oo = [self.aalloc("mo%d" % i, [D], F32, stream=True) for i in range(3)]

        def load_expert(e):
            bi = e % 2
            self.dma("pool", WG[bi], WG[bi].ap, self.w_gate[l, e].rearrange("(k p) n -> p k n", p=P), [], [WG[bi].r])
            self.dma("pool", WU[bi], WU[bi].ap, self.w_up[l, e].rearrange("(k p) n -> p k n", p=P), [], [WU[bi].r])
            self.dma("pool", WD[bi], WD[bi].ap, self.w_down[l, e].rearrange("(k p) n -> p k n", p=P), [], [WD[bi].r])

        load_expert(0)
        def route(ii):
            pl = PB[6 + (ii % 2)]
            for k in range(KD):
                self.mm(pl.ap[:, 0:NE], HT.ap[:, k, ii * P:(ii + 1) * P], self.rwb.ap[:, k, :], k == 0, k == KD - 1,
                        [HT.r, self.rwb.r], [pl.r])
            self.router(pl, gates, ii)

        for ii in range(ns):
            i = i0 + ii
            j = b if i < 32 else 2
            self.lnt_tile(self.xs_d[b, i * P:(i + 1) * P, :], self.xs_r[b][i], HT, ii * P,
                          self.m1p.ap[:, l, 1, :, j], self.mT.ap[:, l, 24:32, j])
            if ii >= 1:
                route(ii - 1)
        route(ns - 1)
        tiles = []
        t0 = 0
        while t0 < ntok:
            n = min(512, ntok - t0)
            tiles.append((t0, n))
            t0 += n
        items = [(e, t0, n) for e in range(NE) for (t0, n) in tiles]
        ycnt = [0]

        def gu(idx):
            e, t0, n = items[idx]
            bi = e % 2
            he = HE[idx % 2]
            for c in range(4):
                pg, pu = PB[2 * (c % 2)], PB[2 * (c % 2) + 1]
                for k in range(KD):
                    self.mm(pg.ap[:, 0:n], WG[bi].ap[:, k, c * P:(c + 1) * P], HT.ap[:, k, t0:t0 + n], k == 0, k == KD - 1,
                            [WG[bi].r, HT.r], [pg.r])
                for k in range(KD):
                    self.mm(pu.ap[:, 0:n], WU[bi].ap[:, k, c * P:(c + 1) * P], HT.ap[:, k, t0:t0 + n], k == 0, k == KD - 1,
                            [WU[bi].r, HT.r], [pu.r])
                sg = SG[c % 2]
                self.act(sg.ap[:, 0:n], pg.ap[:, 0:n], AF.Silu, [pg.r], [sg.r])
                self.tt("dve", he.ap[:, c, 0:n], sg.ap[:, 0:n], pu.ap[:, 0:n], ALU.mult, [sg.r, pu.r], [he.r])

        def down(idx):
            e, t0, n = items[idx]
            bi = e % 2
            he = HE[idx % 2]
            for s in range(n // P):
                ii = t0 // P + s
                for half in (0, 1):
                    py = PB[4 + (ycnt[0] % 4)]
                    ycnt[0] += 1
                    for c in range(4):
                        self.mm(py.ap, he.ap[:, c, s * P:(s + 1) * P], WD[bi].ap[:, c, half * 512:(half + 1) * 512],
                                c == 0, c == 3, [he.r, WD[bi].r], [py.r])
                    dst = ACC.ap[:, ii, half * 512:(half + 1) * 512]
                    g = gates.ap[:, ii, e:e + 1]
                    if e == 0:
                        self.ts("dve", dst, py.ap, g, None, ALU.mult, None, [py.r, gates.r], [acc_r[ii]])
                    else:
                        self.stt("dve", dst, py.ap, g, dst, ALU.mult, ALU.add, [py.r, gates.r, acc_r[ii]], [acc_r[ii]])

        for idx in range(len(items)):
            e, t0, n = items[idx]
            if t0 == 0 and e + 1 < NE:
                load_expert(e + 1)
            if idx == 0:
                gu(0)
            if idx + 1 < len(items):
                gu(idx + 1)
            down(idx)
        for ii in range(ns):
            i = i0 + ii
            j = b if i < 32 else 2
            zi = ii % 3
            xt = self.lnt_xt[zi]
            self.dma("sp", xt, xt.ap, self.xs_d[b, i * P:(i + 1) * P, :], [self.xs_r[b][i]], [xt.r])
            self.tt("pool", z[zi].ap, ACC.ap[:, ii, :], g2bc[j].ap, ALU.mult, [acc_r[ii], g2bc[j].r], [z[zi].r])
            self.stt("dve", z[zi].ap, xt.ap, ALPHA, z[zi].ap, ALU.mult, ALU.add, [xt.r, z[zi].r], [z[zi].r])
            if last:
                dst, dst_r = self.y[b, i * P:(i + 1) * P, :], self.y_r
            else:
                dst, dst_r = self.xs_d[b, i * P:(i + 1) * P, :], self.xs_r[b][i]
            self.post_ln(z[zi], self.stat[zi], zn[zi], oo[zi], lng, lnb, dst, dst_r)

    def router(self, pl, gates, ii):
        t = self.rt[ii % 2]
        R = [t.r]
        a = t.ap
        sc, sel, m1, sel2, m2, gs, gm, ing, msk, oh, w = (a[:, 16 * i:16 * (i + 1)] for i in range(11))
        den = a[:, 176:177]
        self.act(sc, pl.ap[:, 0:NE], AF.Exp, [pl.r], R, scale=-1.0)
        self.ts("dve", sc, sc, 1.0, None, ALU.add, None, R, R)
        self.S.op("dve", lambda e: e.reciprocal(out=sc, in_=sc), reads=R, writes=R) if False else \
            self.S.op("dve", lambda e: e.reciprocal(out=sc, in_=sc), reads=R, writes=R)
        self.tt("dve", sel, sc, self.rbias.ap, ALU.add, R + [self.rbias.r], R)
        g4 = lambda v: v.rearrange("p (g e) -> p g e", g=4)
        self.red("dve", m1[:, 0:4], g4(sel), ALU.max, R, R)
        self.tt("dve", g4(sel2), g4(sel), m1[:, 0:4].unsqueeze(2).broadcast_to([P, 4, 4]), ALU.is_equal, R, R)
        self.stt("dve", sel2, sel2, -BIG, sel, ALU.mult, ALU.add, R, R)
        self.red("dve", m2[:, 0:4], g4(sel2), ALU.max, R, R)
        self.tt("dve", gs[:, 0:4], m1[:, 0:4], m2[:, 0:4], ALU.add, R, R)
        self.red("dve", gm[:, 0:1], gs[:, 0:4], ALU.max, R, R)
        self.ts("dve", ing[:, 0:4], gs[:, 0:4], gm[:, 0:1], None, ALU.is_equal, None, R, R)
        self.ts("dve", ing[:, 0:4], ing[:, 0:4], -1.0, BIG, ALU.add, ALU.mult, R, R)
        self.tt("dve", g4(msk), g4(sel), ing[:, 0:4].unsqueeze(2).broadcast_to([P, 4, 4]), ALU.add, R, R)
        self.red("dve", gm[:, 1:2], msk, ALU.max, R, R)
        self.ts("dve", oh, msk, gm[:, 1:2], None, ALU.is_equal, None, R, R)
        self.stt("dve", msk, oh, -BIG, msk, ALU.mult, ALU.add, R, R)
        self.red("dve", gm[:, 2:3], msk, ALU.max, R, R)
        self.ts("dve", sel2, msk, gm[:, 2:3], None, ALU.is_equal, None, R, R)
        self.tt("dve", oh, oh, sel2, ALU.add, R, R)
        self.tt("dve", w, sc, oh, ALU.mult, R, R)
        self.red("dve", den, w, ALU.add, R, R)
        self.S.op("dve", lambda e: e.reciprocal(out=den, in_=den), reads=R, writes=R)
        self.ts("dve", gates.ap[:, ii, :], w, den, None, ALU.mult, None, R, [gates.r])

    debug_layer = 0

    def dbg_dump_bf16(self, name, buf, nfree):
        self.S.barrier()
        out = self.dbg_out[name]
        tmp = self.aalloc("dbgtmp_" + name, [512], F32)
        flat = buf.ap
        if len(flat.shape) == 3:
            flat = flat.rearrange("p a b -> p (a b)")
        for o in range(0, nfree, 512):
            n = min(512, nfree - o)
            self.cp("dve", tmp.ap[:, 0:n], flat[:, o:o + n], [buf.r], [tmp.r])
            self.dma("sp", self.dbg_stream, out[:, o:o + n], tmp.ap[:, 0:n], [tmp.r], [self.dbg_r])
        self.S.barrier()


def _rope_tables():
    t = np.arange(NLAT, dtype=np.int32)
    row = (t // 64).astype(np.float32)
    col = (t % 64).astype(np.float32)
    inv = (np.float32(1.0) / (np.float32(10000.0) ** (np.arange(16, dtype=np.float32) / np.float32(16)))).astype(np.float32)
    cosT = np.zeros((P, NLAT), np.float32)
    sinT = np.zeros((P, NLAT), np.float32)
    for p in range(P):
        f = p % 64
        axis, half, fr = f // 32, (f // 16) % 2, f % 16
        pos = row if axis == 0 else col
        ang = (pos * inv[fr]).astype(np.float32)
        cosT[p] = np.cos(ang)
        sinT[p] = np.sin(ang) * (-1.0 if half == 0 else 1.0)
    return cosT, sinT


def _host_inputs(inputs):
    f = lambda k: np.ascontiguousarray(np.asarray(inputs[k], dtype=np.float32))
    w_in = f("w_in")
    cols = np.arange(512)
    perm = cols ^ 16
    q, k_ = w_in[:, :, 0:512], w_in[:, :, 512:1024]
    w_inx = np.ascontiguousarray(np.concatenate(
        [q, q[:, :, perm], k_, k_[:, :, perm], w_in[:, :, 1024:1536], w_in[:, :, 1536:2048], w_in[:, :, 2048:2560],
         w_in[:, :, 2560:3072]], axis=2))
    cosT, sinT = _rope_tables()
    shared = {
        "w_mod": f("w_mod"), "b_mod": f("b_mod"), "w_inx": w_inx,
        "dl": np.ascontiguousarray(f("diff_lambda").reshape(1, -1)),
        "gT": np.ascontiguousarray(f("attn_norm_g").reshape(DEPTH, NH, P).transpose(2, 0, 1)),
        "cwT": np.ascontiguousarray(f("conv_w").reshape(DEPTH, 3, 4, P).transpose(3, 0, 1, 2)),
        "cbT": np.ascontiguousarray(f("conv_b").reshape(DEPTH, 4, P).transpose(2, 0, 1)),
        "w_out": f("w_out"), "ln1_g": f("ln1_g"), "ln1_b": f("ln1_b"), "ln2_g": f("ln2_g"), "ln2_b": f("ln2_b"),
        "router_w": f("router_w"), "router_bias": np.ascontiguousarray(f("router_bias").reshape(1, NE)),
        "w_gate": f("w_gate"), "w_up": f("w_up"), "w_down": f("w_down"),
        "ident": np.eye(P, dtype=np.float32), "cosT": cosT, "sinT": sinT,
    }
    x, c, ctx, c_ctx = f("x"), f("c"), f("ctx"), f("c_ctx")
    in_maps = []
    for i in range(NCORES):
        cc = np.stack([c[2 * i], c[2 * i + 1], c_ctx], axis=0)
        ccT = np.ascontiguousarray(cc.reshape(3, KD, P).transpose(2, 1, 0))
        m = dict(shared)
        m["x"] = np.ascontiguousarray(x[2 * i:2 * i + 2])
        m["ctx"] = np.ascontiguousarray(ctx[2 * i:2 * i + 2])
        m["ccT"] = ccT
        in_maps.append(m)
    return in_maps


_NC_CACHE = {}


def kernel(**inputs):
    in_maps = _host_inputs(inputs)
    if "nc" not in _NC_CACHE:
        _NC_CACHE["nc"] = Builder().build()
    res = run_bass_kernel_spmd(_NC_CACHE["nc"], in_maps, core_ids=list(range(NCORES)))
    return np.concatenate([np.asarray(r["y"], dtype=np.float32) for r in res.results], axis=0)
```

```python
import math
from contextlib import ExitStack

import numpy as np

import concourse.bass as bass
import concourse.mybir as mybir
from concourse.bass_utils import run_bass_kernel_spmd

F32 = mybir.dt.float32
BF16 = mybir.dt.bfloat16
AF = mybir.ActivationFunctionType
ALU = mybir.AluOpType

P = 128
D = 1024
KD = 8
NLAT = 4096
NCTX = 256
NTOK = NLAT + NCTX
NT = NTOK // P
NH = 4
NE = 16
DEXP = 512
DEPTH = 2
ALPHA = (2 * DEPTH) ** 0.25
LN_EPS = 1e-6
HEAD_EPS = 1e-5
QSCALE = 64 ** -0.5
NCORES = 8
BIG = 100.0


class Res:
    __slots__ = ("name", "w", "r", "excl")

    def __init__(self, name, excl=False):
        self.name = name
        self.w = None
        self.r = {}
        self.excl = excl


class Stream:
    __slots__ = ("sem", "count")

    def __init__(self, sem):
        self.sem = sem
        self.count = 0


class EngQ:
    def __init__(self, name, sem, inorder):
        self.name = name
        self.sem = sem
        self.count = 0
        self.waited = {}
        self.ops = []
        self.inorder = inorder


class Sched:
    def __init__(self, nc, es):
        self.nc = nc
        self.es = es
        self.q = {}
        for name, inorder in (("pe", True), ("act", False), ("dve", False), ("pool", False), ("sp", True)):
            sem = es.enter_context(nc.semaphore("q_" + name))
            self.q[name] = EngQ(name, sem, inorder)
        self.streams = []

    def stream(self):
        s = Stream(self.es.enter_context(self.nc.semaphore("d%d" % len(self.streams))))
        self.streams.append(s)
        return s

    def _emit(self, eng, fn, reads, writes, stream):
        E = self.q[eng]
        waits = {}

        def need(sv):
            if sv is None:
                return
            s, v = sv
            if s is E.sem and E.inorder and stream is None:
                return
            k = id(s)
            if E.waited.get(k, 0) >= v:
                return
            if k not in waits or waits[k][1] < v:
                waits[k] = (s, v)

        for r in reads:
            need(r.w)
            if r.excl:
                for sv in r.r.values():
                    if sv[0] is not E.sem:
                        need(sv)
        for w in writes:
            need(w.w)
            for sv in w.r.values():
                need(sv)
        if stream is not None and stream.count:
            need((stream.sem, stream.count))
        for k, (s, v) in waits.items():
            E.waited[k] = v
        if stream is None:
            E.count += 1
            done = (E.sem, E.count)
            inc = 1
        else:
            stream.count += 16
            done = (stream.sem, stream.count)
            inc = 16
        E.ops.append((list(waits.values()), fn, done[0], inc))
        k = id(done[0])
        for r in reads:
            r.r[k] = done
        for w in writes:
            w.w = done
            w.r = {}
        return done

    def op(self, eng, fn, reads=(), writes=()):
        return self._emit(eng, fn, reads, writes, None)

    def dma(self, eng, stream, out, in_, reads=(), writes=()):
        return self._emit(eng, lambda e: e.dma_start(out=out, in_=in_), reads, writes, stream)

    def barrier(self, engines=("pe", "act", "dve", "pool", "sp")):
        targets = [(E.sem, E.count) for E in self.q.values() if E.count]
        targets += [(s.sem, s.count) for s in self.streams if s.count]
        for name in engines:
            E = self.q[name]
            waits = []
            for s, v in targets:
                if s is E.sem and E.inorder:
                    continue
                k = id(s)
                if E.waited.get(k, 0) >= v:
                    continue
                E.waited[k] = v
                waits.append((s, v))
            if waits:
                E.ops.append((waits, None, None, 0))

    def replay(self, block):
        def run(E):
            def body(eng):
                for waits, fn, sem, inc in E.ops:
                    for s, v in waits:
                        eng.wait_ge(s, v)
                    if fn is not None:
                        fn(eng).then_inc(sem, inc)
            return body

        block.tensor(run(self.q["pe"]))
        block.scalar(run(self.q["act"]))
        block.vector(run(self.q["dve"]))
        block.gpsimd(run(self.q["pool"]))
        block.sync(run(self.q["sp"]))


class Buf:
    __slots__ = ("ap", "r", "st", "stp")

    def __init__(self, ap, r, st=None):
        self.ap = ap
        self.r = r
        self.st = st
        self.stp = None


class Builder:
    def __init__(self, layers=(0, 1), debug=None, nbatch=2, stop_after=None, scratch_out=False, nb_run=None):
        self.layers = tuple(layers)
        self.debug = debug or {}
        self.nbatch = nbatch
        self.stop_after = stop_after
        self.scratch_out = scratch_out
        self.nb_run = nb_run if nb_run is not None else nbatch

    def mm(self, out, lhsT, rhs, start, stop, R, W, skip=False):
        self.S.op("pe", lambda e: e.matmul(out, lhsT=lhsT, rhs=rhs, start=start, stop=stop,
                                            skip_group_check=skip), reads=R, writes=W)

    def tr(self, out, in_, ident, R, W):
        self.S.op("pe", lambda e: e.transpose(out, in_, ident), reads=R, writes=W)

    def act(self, out, in_, func, R, W, scale=1.0, bias=0.0):
        self.S.op("act", lambda e: e.activation(out=out, in_=in_, func=func, bias=bias, scale=scale),
                  reads=R, writes=W)

    def tt(self, eng, out, in0, in1, op, R, W):
        self.S.op(eng, lambda e: e.tensor_tensor(out=out, in0=in0, in1=in1, op=op), reads=R, writes=W)

    def ts(self, eng, out, in0, s1, s2, op0, op1, R, W):
        if s2 is None:
            self.S.op(eng, lambda e: e.tensor_scalar(out=out, in0=in0, scalar1=s1, scalar2=None, op0=op0),
                      reads=R, writes=W)
        else:
            self.S.op(eng, lambda e: e.tensor_scalar(out=out, in0=in0, scalar1=s1, scalar2=s2, op0=op0, op1=op1),
                      reads=R, writes=W)

    def stt(self, eng, out, in0, scalar, in1, op0, op1, R, W):
        self.S.op(eng, lambda e: e.scalar_tensor_tensor(out=out, in0=in0, scalar=scalar, in1=in1, op0=op0, op1=op1),
                  reads=R, writes=W)

    def cp(self, eng, out, in_, R, W):
        self.S.op(eng, lambda e: e.tensor_copy(out=out, in_=in_), reads=R, writes=W)

    def red(self, eng, out, in_, op, R, W):
        self.S.op(eng, lambda e: e.tensor_reduce(out=out, in_=in_, axis=mybir.AxisListType.X, op=op),
                  reads=R, writes=W)

    def memset(self, eng, ap, val, W):
        self.S.op(eng, lambda e: e.memset(ap, val), writes=W)

    def dma(self, eng, buf_or_stream, out, in_, R, W):
        if isinstance(buf_or_stream, Buf):
            if eng == "pool":
                if buf_or_stream.stp is None:
                    if self.ppool_i >= len(self.ppool):
                        self.ppool.append(self.S.stream())
                    buf_or_stream.stp = self.ppool[self.ppool_i]
                    self.ppool_i += 1
                st = buf_or_stream.stp
            else:
                st = buf_or_stream.st
        else:
            st = buf_or_stream
        self.S.dma(eng, st, out, in_, reads=R, writes=W)

    def sb(self, name, shape, dtype, stream=False):
        t = self.es.enter_context(self.nc.sbuf_tensor("s_" + name, list(shape), dtype))
        return Buf(t[:], Res(name), self.S.stream() if stream else None)

    def arena_reset(self):
        self.aoff = 0

    def aalloc(self, name, free_shape, dtype, stream=False):
        n = 1
        for s in free_shape:
            n *= s
        nb16 = n * (2 if dtype == F32 else 1)
        nb16 = (nb16 + 15) // 16 * 16
        assert self.aoff + nb16 <= self.asize, (name, self.aoff, nb16, self.asize)
        v = self.arena[:, self.aoff:self.aoff + n * (2 if dtype == F32 else 1)]
        self.aoff += nb16
        if dtype == F32:
            v = v.bitcast(F32)
        if len(free_shape) == 2:
            v = v.rearrange("p (a b) -> p a b", a=free_shape[0])
        elif len(free_shape) == 3:
            v = v.rearrange("p (a b c) -> p a b c", a=free_shape[0], b=free_shape[1])
        st = None
        if stream:
            if self.spool_i >= len(self.spool):
                self.spool.append(self.S.stream())
            st = self.spool[self.spool_i]
            self.spool_i += 1
        return Buf(v, Res(name), st)

    def phase_begin(self):
        self.S.barrier()
        self.arena_reset()
        self.spool_i = 0
        self.ppool_i = 0

    def build(self):
        nc = bass.Bass("TRN2", target_bir_lowering=False)
        self.nc = nc
        NB = self.nbatch
        din = lambda name, shape, dt=F32: nc.dram_tensor(name, list(shape), dt, kind="ExternalInput").ap()
        self.x_in = din("x", [NB, NLAT, D])
        self.ctx_in = din("ctx", [NB, NCTX, D])
        self.ccT_in = din("ccT", [P, KD, 3])
        self.w_mod = din("w_mod", [DEPTH, D, 6 * D])
        self.b_mod = din("b_mod", [DEPTH, 6 * D])
        self.w_inx = din("w_inx", [DEPTH, D, 4096])
        self.dl_in = din("dl", [1, DEPTH * 4 * 64])
        self.gT_in = din("gT", [P, DEPTH, NH])
        self.cwT_in = din("cwT", [P, DEPTH, 3, 4])
        self.cbT_in = din("cbT", [P, DEPTH, 4])
        self.w_out = din("w_out", [DEPTH, D, D])
        self.ln1_g = din("ln1_g", [DEPTH, D])
        self.ln1_b = din("ln1_b", [DEPTH, D])
        self.ln2_g = din("ln2_g", [DEPTH, D])
        self.ln2_b = din("ln2_b", [DEPTH, D])
        self.router_w = din("router_w", [D, NE])
        self.router_bias = din("router_bias", [1, NE])
        self.w_gate = din("w_gate", [DEPTH, NE, D, DEXP])
        self.w_up = din("w_up", [DEPTH, NE, D, DEXP])
        self.w_down = din("w_down", [DEPTH, NE, DEXP, D])
        self.ident_in = din("ident", [P, P])
        self.cos_in = din("cosT", [P, NLAT])
        self.sin_in = din("sinT", [P, NLAT])
        self.y = nc.dram_tensor("y", [NB, NLAT, D], F32, kind="ExternalOutput").ap()
        self.dbg_out = {}
        for name, shape in self.debug.items():
            self.dbg_out[name] = nc.dram_tensor("dbg_" + name, list(shape), F32, kind="ExternalOutput").ap()
        sk = {"kind": "ExternalOutput"} if self.scratch_out else {}
        self.xs_d = nc.dram_tensor("xs_d", [NB, NTOK, D], F32, **sk).ap()
        self.QT_d = nc.dram_tensor("QT_d", [NH, P, NTOK], BF16, **sk).ap()
        self.KT_d = nc.dram_tensor("KT_d", [NH, P, NTOK], BF16, **sk).ap()
        self.VS_d = nc.dram_tensor("VS_d", [NTOK, 512], BF16, **sk).ap()
        self.CV_d = nc.dram_tensor("CV_d", [4, P, NTOK], BF16, **sk).ap()
        self.mrow_d = nc.dram_tensor("mrow_d", [DEPTH, 3, 6 * D], F32, **sk).ap()
        self.xs_r = [[Res("xs%d_%d" % (b, i)) for i in range(NT)] for b in range(NB)]
        self.QT_r = [[Res("qt%d_%d" % (h, j)) for j in range(9)] for h in range(NH)]
        self.KT_r = [[Res("kt%d_%d" % (h, j)) for j in range(9)] for h in range(NH)]
        self.VS_r = [Res("vs%d" % i) for i in range(NT)]
        self.CV_r = [[Res("cv%d_%d" % (c, j)) for j in range(3)] for c in range(4)]
        self.mrow_r = [Res("mrowd%d" % l) for l in range(DEPTH)]
        self.y_r = Res("y")
        self.dbg_r = Res("dbg")

        with ExitStack() as es:
            self.es = es
            self.S = Sched(nc, es)
            S = self.S
            self.PB = []
            for i in range(8):
                t = es.enter_context(nc.psum_tensor("pb%d" % i, [P, 512], F32))
                self.PB.append(Buf(t[:], Res("pb%d" % i, excl=True)))
            self.identf = self.sb("identf", [P, P], F32, stream=True)
            self.identb = self.sb("identb", [P, P], BF16)
            self.onesb = self.sb("onesb", [P, P], BF16)
            self.onesf = self.sb("onesf", [P, P], F32)
            self.scT = self.sb("scT", [P, KD, 3], F32, stream=True)
            self.mT = self.sb("mT", [P, DEPTH, 48, 3], F32)
            self.m1p = self.sb("m1p", [P, DEPTH, 2, KD, 3], F32)
            self.dl = self.sb("dl", [P, DEPTH * 4 * 64], F32, stream=True)
            self.lamt = self.sb("lamt", [P, 8], F32)
            self.nlam = self.sb("nlam", [P, DEPTH], F32)
            self.gT = self.sb("gTs", [P, DEPTH, NH], F32, stream=True)
            self.gsc = self.sb("gsc", [P, DEPTH, NH], F32)
            self.cw = self.sb("cw", [P, DEPTH, 3, 4], F32, stream=True)
            self.cb = self.sb("cbs", [P, DEPTH, 4], F32, stream=True)
            self.rwb = self.sb("rwb", [P, KD, NE], BF16, stream=True)
            self.rbias = self.sb("rbias", [P, NE], F32, stream=True)
            self.stat = [self.sb("stat%d" % i, [P, 16], F32) for i in range(3)]
            self.rt = [self.sb("rt%d" % i, [P, 16 * 12], F32) for i in range(2)]
            self.asize = 99 * 1024
            arena_t = es.enter_context(nc.sbuf_tensor("arena", [P, self.asize], BF16))
            self.arena = arena_t[:]
            self.spool = []
            self.spool_i = 0
            self.ppool = []
            self.ppool_i = 0
            self.arena_reset()
            self.dbg_stream = S.stream()

            self.setup_phase()
            first = True
            stop = self.stop_after
            done = False
            for l in self.layers:
                last = l == DEPTH - 1
                self.mod_phase(l)
                if stop == "mod":
                    break
                for b in range(self.nb_run):
                    self.lnt_phase_and_inproj(l, b, first, last)
                    if stop in ("lnt", "inproj"):
                        done = True
                        break
                    self.att_phase(l, b, first, last)
                    if stop in ("att", "att_q0", "att_q0b", "att_q1b"):
                        done = True
                        break
                    self.moe_phase(l, b, last)
                if done:
                    break
                first = False
            S.barrier()
            with nc.Block() as block:
                S.replay(block)
        return nc

    def xsrc(self, first, b, i):
        if first:
            if i < 32:
                return self.x_in[b, i * P:(i + 1) * P, :], None
            return self.ctx_in[b, (i - 32) * P:(i - 31) * P, :], None
        return self.xs_d[b, i * P:(i + 1) * P, :], self.xs_r[b][i]

    def setup_phase(self):
        S = self.S
        self.dma("sp", self.identf, self.identf.ap, self.ident_in, [], [self.identf.r])
        self.cp("dve", self.identb.ap, self.identf.ap, [self.identf.r], [self.identb.r])
        self.memset("pool", self.onesb.ap, 1.0, [self.onesb.r])
        self.memset("pool", self.onesf.ap, 1.0, [self.onesf.r])
        self.dma("sp", self.scT, self.scT.ap, self.ccT_in, [], [self.scT.r])
        self.act(self.scT.ap, self.scT.ap, AF.Silu, [self.scT.r], [self.scT.r])
        self.dma("sp", self.dl, self.dl.ap, self.dl_in.broadcast_to([P, DEPTH * 4 * 64]), [], [self.dl.r])
        self.dma("sp", self.gT, self.gT.ap, self.gT_in, [], [self.gT.r])
        self.dma("sp", self.cw, self.cw.ap, self.cwT_in, [], [self.cw.r])
        self.dma("sp", self.cb, self.cb.ap, self.cbT_in, [], [self.cb.r])
        self.dma("pool", self.rwb, self.rwb.ap, self.router_w.rearrange("(k p) e -> p k e", p=P), [], [self.rwb.r])
        self.dma("sp", self.rbias, self.rbias.ap, self.router_bias.broadcast_to([P, NE]), [], [self.rbias.r])
        dl4 = self.dl.ap.rearrange("p (l j d) -> p l j d", l=DEPTH, j=4)
        lt = self.lamt
        for l in range(DEPTH):
            lam_init = 0.8 - 0.6 * math.exp(-0.3 * l)
            tmp = self.rt[0]
            for j in range(2):
                self.tt("dve", tmp.ap[:, 0:64], dl4[:, l, 2 * j, :], dl4[:, l, 2 * j + 1, :], ALU.mult,
                        [self.dl.r], [tmp.r])
                self.red("dve", lt.ap[:, 4 * l + j:4 * l + j + 1], tmp.ap[:, 0:64], ALU.add, [tmp.r], [lt.r])
            self.act(lt.ap[:, 4 * l:4 * l + 2], lt.ap[:, 4 * l:4 * l + 2], AF.Exp, [lt.r], [lt.r])
            self.tt("dve", lt.ap[:, 4 * l + 2:4 * l + 3], lt.ap[:, 4 * l + 1:4 * l + 2], lt.ap[:, 4 * l:4 * l + 1],
                    ALU.subtract, [lt.r], [lt.r])
            self.ts("dve", self.nlam.ap[:, l:l + 1], lt.ap[:, 4 * l + 2:4 * l + 3], -lam_init, None, ALU.add, None,
                    [lt.r], [self.nlam.r])
            self.ts("dve", self.gsc.ap[:, l, :], self.gT.ap[:, l, :], 1.0 - lam_init, None, ALU.mult, None,
                    [self.gT.r], [self.gsc.r])

    def mod_phase(self, l):
        self.phase_begin()
        wm = [self.aalloc("wm%d" % i, [KD, 512], F32, stream=True) for i in range(2)]
        mrow = self.aalloc("mrow", [6 * D], F32, stream=True)
        bm3 = self.aalloc("bm3", [6 * D], F32, stream=True)
        self.dma("sp", bm3, bm3.ap[0:3, :], self.b_mod[l:l + 1, :].broadcast_to([3, 6 * D]), [], [bm3.r])
        for cb in range(12):
            w = wm[cb % 2]
            self.dma("sp", w, w.ap, self.w_mod[l, :, cb * 512:(cb + 1) * 512].rearrange("(k p) n -> p k n", p=P),
                     [], [w.r])
            bank = self.PB[cb % 2]
            for k in range(KD):
                self.mm(bank.ap[0:3, :], self.scT.ap[:, k, :], w.ap[:, k, :], k == 0, k == KD - 1,
                        [self.scT.r, w.r], [bank.r])
            self.tt("dve", mrow.ap[0:3, cb * 512:(cb + 1) * 512], bank.ap[0:3, :], bm3.ap[0:3, cb * 512:(cb + 1) * 512],
                    ALU.add, [bank.r, bm3.r], [mrow.r])
        self.dma("sp", mrow, self.mrow_d[l], mrow.ap[0:3, :], [mrow.r], [self.mrow_r[l]])
        pT = self.PB[2]
        pTv = pT.ap[:, 0:144].rearrange("p (j c) -> p j c", c=3)
        for jn in range(48):
            self.tr(pTv[:, jn, :], mrow.ap[0:3, jn * P:(jn + 1) * P], self.identf.ap[0:3, 0:3],
                    [mrow.r, self.identf.r], [pT.r])
        self.cp("dve", self.mT.ap[:, l, :, :], pTv, [pT.r], [self.mT.r])
        for w_, v in ((0, 1), (1, 4)):
            self.ts("dve", self.m1p.ap[:, l, w_, :, :], self.mT.ap[:, l, v * 8:(v + 1) * 8, :], 1.0, None, ALU.add, None,
                    [self.mT.r], [self.m1p.r])

    def lnt_setup(self):
        self.lnt_xt = [self.aalloc("lxt%d" % i, [D], F32, stream=True) for i in range(3)]
        self.lnt_xn = [self.aalloc("lxn%d" % i, [D], BF16) for i in range(3)]
        self.lnt_cnt = 0

    def ln_stats(self, z, st):
        self.S.op("dve", lambda e: e.bn_stats(st.ap[:, 0:6], z.ap[:, 0:512]), reads=[z.r], writes=[st.r])
        self.S.op("dve", lambda e: e.bn_stats(st.ap[:, 6:12], z.ap[:, 512:1024]), reads=[z.r], writes=[st.r])
        self.S.op("dve", lambda e: e.bn_aggr(st.ap[:, 12:14], st.ap[:, 0:12]), reads=[st.r], writes=[st.r])
        self.act(st.ap[:, 14:15], st.ap[:, 13:14], AF.Ln, [st.r], [st.r], bias=LN_EPS)
        self.act(st.ap[:, 14:15], st.ap[:, 14:15], AF.Exp, [st.r], [st.r], scale=-0.5)
        self.stt("dve", st.ap[:, 15:16], st.ap[:, 12:13], -1.0, st.ap[:, 14:15], ALU.mult, ALU.mult, [st.r], [st.r])

    def lnt_tile(self, src_ap, src_r, UT, tok_off, scp, shv):
        c = self.lnt_cnt
        self.lnt_cnt += 1
        bi = c % 3
        xt, xn, st = self.lnt_xt[bi], self.lnt_xn[bi], self.stat[bi]
        self.dma("sp", xt, xt.ap, src_ap, [src_r] if src_r is not None else [], [xt.r])
        self.ln_stats(xt, st)
        self.ts("dve", xn.ap, xt.ap, st.ap[:, 12:13], st.ap[:, 14:15], ALU.subtract, ALU.mult, [xt.r, st.r], [xn.r])
        pa, pb = self.PB[2 * bi], self.PB[2 * bi + 1]
        for half, bank in ((0, pa), (1, pb)):
            pv = bank.ap.bitcast(BF16).rearrange("p (k t) -> p k t", k=8)
            for kk in range(4):
                k = half * 4 + kk
                self.tr(pv[:, kk, :], xn.ap[:, k * P:(k + 1) * P], self.identb.ap, [xn.r, self.identb.r], [bank.r])
            for kk in range(4):
                k = half * 4 + kk
                dst = UT.ap[:, k, tok_off:tok_off + P]
                if half == 0:
                    self.act(dst, pv[:, kk, :], AF.Identity, [bank.r, self.m1p.r, self.mT.r], [UT.r],
                             scale=scp[:, k:k + 1], bias=shv[:, k:k + 1])
                else:
                    self.ts("dve", dst, pv[:, kk, :], scp[:, k:k + 1], shv[:, k:k + 1], ALU.mult, ALU.add,
                            [bank.r, self.m1p.r, self.mT.r], [UT.r])

    def lnt_phase_and_inproj(self, l, b, first, last):
        self.phase_begin()
        UT = self.aalloc("UT", [KD, NTOK], BF16)
        self.lnt_setup()
        for i in range(NT):
            j = b if i < 32 else 2
            src, src_r = self.xsrc(first, b, i)
            self.lnt_tile(src, src_r, UT, i * P, self.m1p.ap[:, l, 0, :, j], self.mT.ap[:, l, 0:8, j])
        if "uT" in self.dbg_out and l == self.debug_layer and b == 0:
            self.dbg_dump_bf16("uT", UT, KD * NTOK)
        if self.stop_after == "lnt":
            return
        wA = self.aalloc("wA", [KD, 512], BF16, stream=True)
        wB = self.aalloc("wB", [KD, 512], BF16, stream=True)
        wC = self.aalloc("wC", [KD, 512], BF16, stream=True)
        mark = self.aoff
        cosT = self.aalloc("cosT", [NLAT], F32, stream=True)
        sinT = self.aalloc("sinT", [NLAT], F32, stream=True)
        t1 = [self.aalloc("rt1_%d" % i, [512], F32) for i in range(2)]
        t2 = [self.aalloc("rt2_%d" % i, [512], F32) for i in range(2)]
        qo = [self.aalloc("qo%d" % i, [512], BF16, stream=True) for i in range(2)]
        self.dma("sp", cosT, cosT.ap, self.cos_in, [], [cosT.r])
        self.dma("sp", sinT, sinT.ap, self.sin_in, [], [sinT.r])
        wx = self.w_inx[l].rearrange("(k p) n -> p k n", p=P)
        cnt = 0
        for which, base, dst, dst_r in (("q", 0, self.QT_d, self.QT_r), ("k", 1024, self.KT_d, self.KT_r)):
            self.dma("pool", wA, wA.ap, wx[:, :, base:base + 512], [], [wA.r])
            self.dma("pool", wB, wB.ap, wx[:, :, base + 512:base + 1024], [], [wB.r])
            for h in range(NH):
                for jt in range(9):
                    ctx_tile = jt == 8
                    if ctx_tile and which == "q" and last:
                        continue
                    tok0 = jt * 512
                    n = 256 if ctx_tile else 512
                    bi = cnt % 2
                    cnt += 1
                    pa, pb = self.PB[4 + bi], self.PB[6 + bi]
                    for k in range(KD):
                        self.mm(pa.ap[:, 0:n], wA.ap[:, k, h * P:(h + 1) * P], UT.ap[:, k, tok0:tok0 + n],
                                k == 0, k == KD - 1, [wA.r, UT.r], [pa.r])
                    o = qo[bi]
                    if not ctx_tile:
                        for k in range(KD):
                            self.mm(pb.ap[:, 0:n], wB.ap[:, k, h * P:(h + 1) * P], UT.ap[:, k, tok0:tok0 + n],
                                    k == 0, k == KD - 1, [wB.r, UT.r], [pb.r])
                        self.tt("dve", t1[bi].ap, pa.ap, cosT.ap[:, tok0:tok0 + n], ALU.mult, [pa.r, cosT.r], [t1[bi].r])
                        self.tt("dve", t2[bi].ap, pb.ap, sinT.ap[:, tok0:tok0 + n], ALU.mult, [pb.r, sinT.r], [t2[bi].r])
                        self.tt("pool", o.ap, t1[bi].ap, t2[bi].ap, ALU.add, [t1[bi].r, t2[bi].r], [o.r])
                    else:
                        self.act(o.ap[:, 0:n], pa.ap[:, 0:n], AF.Copy, [pa.r], [o.r])
                    self.dma("sp", o, dst[h, :, tok0:tok0 + n], o.ap[:, 0:n], [o.r], [dst_r[h][jt]])
        self.dma("pool", wC, wC.ap, wx[:, :, 2048:2560], [], [wC.r])
        vo = qo
        for i in range(NT):
            bi = i % 2
            pv = self.PB[4 + bi]
            for k in range(KD):
                self.mm(pv.ap, UT.ap[:, k, i * P:(i + 1) * P], wC.ap[:, k, :], k == 0, k == KD - 1, [UT.r, wC.r], [pv.r])
            self.act(vo[bi].ap, pv.ap, AF.Copy, [pv.r], [vo[bi].r])
            self.dma("sp", vo[bi], self.VS_d[i * P:(i + 1) * P, :], vo[bi].ap, [vo[bi].r], [self.VS_r[i]])
        self.S.barrier()
        self.aoff = mark
        GL = NTOK + 8
        G = self.aalloc("G", [GL], F32)
        GB = self.aalloc("GB", [NTOK], F32)
        tmpc = [self.aalloc("tmpc%d" % i, [512], F32) for i in range(2)]
        T = [self.aalloc("T%d" % i, [2048], F32) for i in range(2)]
        Y = [self.aalloc("Y%d" % i, [2048], BF16, stream=True) for i in range(2)]
        self.dma("pool", wA, wA.ap, wx[:, :, 2560:3072], [], [wA.r])
        self.dma("pool", wB, wB.ap, wx[:, :, 3072:3584], [], [wB.r])
        self.dma("pool", wC, wC.ap, wx[:, :, 3584:4096], [], [wC.r])
        self.memset("pool", G.ap[:, 0:1], 0.0, [G.r])
        self.memset("pool", G.ap[:, 4097:4099], 0.0, [G.r])
        self.memset("pool", G.ap[:, 4355:4356], 0.0, [G.r])

        def gidx(t):
            return 1 + t if t < NLAT else 4099 + (t - NLAT)

        ntile = 8 if last else 9
        pieces = [(0, 2048, 0), (2048, 2048, 1)] + ([] if last else [(NLAT, NCTX, 2)])
        ycnt = 0
        for c in range(4):
            for jt in range(ntile):
                tok0 = jt * 512
                n = 256 if jt == 8 else 512
                bi = jt % 2
                pg = [self.PB[bi * 3 + x] for x in range(3)]
                for x, w in enumerate((wA, wB, wC)):
                    for k in range(KD):
                        self.mm(pg[x].ap[:, 0:n], w.ap[:, k, c * P:(c + 1) * P], UT.ap[:, k, tok0:tok0 + n],
                                k == 0, k == KD - 1, [w.r, UT.r], [pg[x].r])
                self.act(GB.ap[:, tok0:tok0 + n], pg[0].ap[:, 0:n], AF.Copy, [pg[0].r], [GB.r])
                self.act(tmpc[bi].ap[:, 0:n], pg[1].ap[:, 0:n], AF.Copy, [pg[1].r], [tmpc[bi].r])
                g0 = gidx(tok0)
                self.tt("dve", G.ap[:, g0:g0 + n], tmpc[bi].ap[:, 0:n], pg[2].ap[:, 0:n], ALU.mult,
                        [tmpc[bi].r, pg[2].r], [G.r])
            for (t0, n, pj) in pieces:
                bi = ycnt % 2
                ycnt += 1
                g0 = gidx(t0)
                Tb, Yb = T[bi], Y[bi]
                self.act(Tb.ap[:, 0:n], G.ap[:, g0:g0 + n], AF.Identity, [G.r, self.cw.r, self.cb.r], [Tb.r],
                         scale=self.cw.ap[:, l, 1, c:c + 1], bias=self.cb.ap[:, l, c:c + 1])
                self.stt("dve", Tb.ap[:, 0:n], G.ap[:, g0 - 1:g0 - 1 + n], self.cw.ap[:, l, 0, c:c + 1], Tb.ap[:, 0:n],
                         ALU.mult, ALU.add, [G.r, self.cw.r, Tb.r], [Tb.r])
                self.stt("dve", Tb.ap[:, 0:n], G.ap[:, g0 + 1:g0 + 1 + n], self.cw.ap[:, l, 2, c:c + 1], Tb.ap[:, 0:n],
                         ALU.mult, ALU.add, [G.r, self.cw.r, Tb.r], [Tb.r])
                self.tt("pool", Yb.ap[:, 0:n], GB.ap[:, t0:t0 + n], Tb.ap[:, 0:n], ALU.mult, [GB.r, Tb.r], [Yb.r])
                self.dma("sp", Yb, self.CV_d[c, :, t0:t0 + n], Yb.ap[:, 0:n], [Yb.r], [self.CV_r[c][pj]])

    def post_ln(self, z, st, zn, o, lng, lnb, dst_ap, dst_r):
        self.ln_stats(z, st)
        self.stt("dve", z.ap, z.ap, st.ap[:, 12:13], lng.ap, ALU.subtract, ALU.mult, [z.r, st.r, lng.r], [z.r])
        self.stt("dve", o.ap, z.ap, st.ap[:, 14:15], lnb.ap, ALU.mult, ALU.add, [z.r, st.r, lnb.r], [o.r])
        self.dma("sp", o, dst_ap, o.ap, [o.r], [dst_r])

    def att_phase(self, l, b, first, last):
        self.phase_begin()
        KT = self.aalloc("KT", [NH, NTOK], BF16, stream=True)
        V = self.aalloc("V", [NT, 512], BF16, stream=True)
        WO = self.aalloc("WO", [KD, D], BF16, stream=True)
        g1bc = {}
        for j in ([b] if last else [b, 2]):
            g1bc[j] = self.aalloc("g1bc%d" % j, [D], F32, stream=True)
            self.dma("sp", g1bc[j], g1bc[j].ap, self.mrow_d[l, j:j + 1, 2 * D:3 * D].broadcast_to([P, D]),
                     [self.mrow_r[l]], [g1bc[j].r])
        lng = self.aalloc("lng", [D], F32, stream=True)
        lnb = self.aalloc("lnb", [D], F32, stream=True)
        self.dma("sp", lng, lng.ap, self.ln1_g[l:l + 1, :].broadcast_to([P, D]), [], [lng.r])
        self.dma("sp", lnb, lnb.ap, self.ln1_b[l:l + 1, :].broadcast_to([P, D]), [], [lnb.r])
        self.dma("sp", KT, KT.ap, self.KT_d.rearrange("h p t -> p h t"),
                 [r for rr in self.KT_r for r in rr], [KT.r])
        self.dma("sp", V, V.ap, self.VS_d.rearrange("(i p) c -> p i c", p=P), list(self.VS_r), [V.r])
        self.dma("pool", WO, WO.ap, self.w_out[l].rearrange("(k p) n -> p k n", p=P), [], [WO.r])
        Qz = [[self.aalloc("Qz%d_%d" % (m, i), [NH, 512], BF16, stream=True) for i in range(2)] for m in range(2)]
        for i in range(2):
            self.memset("pool", Qz[0][i].ap[64:128, :, :], 0.0, [Qz[0][i].r])
            self.memset("pool", Qz[1][i].ap[0:64, :, :], 0.0, [Qz[1][i].r])
        CVt = [self.aalloc("CVt%d" % i, [4, 512], BF16, stream=True) for i in range(2)]
        AT = [self.aalloc("AT%d" % i, [NH, 512], BF16) for i in range(2)]
        ET = [[self.aalloc("ET%d_%d" % (m, i), [512], BF16) for i in range(4)] for m in range(2)]
        EA1 = [self.aalloc("EA1_%d" % i, [512], F32) for i in range(2)]
        O12 = [self.aalloc("O12_%d" % m, [512], F32) for m in range(2)]
        S0c = self.aalloc("S0c", [512], F32)
        r12 = [self.aalloc("r%d" % m, [512], F32) for m in range(2)]
        a12 = [self.aalloc("a%d" % m, [512], F32) for m in range(2)]
        vv = [self.aalloc("vv%d" % i, [512], F32) for i in range(2)]
        sq = [self.aalloc("sq%d" % i, [512], F32) for i in range(2)]
        lnr = [self.aalloc("lnr%d" % i, [512], F32) for i in range(2)]
        xt = [self.aalloc("axt%d" % i, [D], F32, stream=True) for i in range(2)]
        z = [self.aalloc("az%d" % i, [D], F32) for i in range(2)]
        zn = z
        oo = [self.aalloc("ao%d" % i, [D], F32, stream=True) for i in range(2)]
        PB = self.PB
        ecnt = [0, 0]
        hcnt = 0
        ocnt = 0
        scnt = 0
        nqt = 8 if last else 9

        def load_q(qt):
            n = 256 if qt == 8 else 512
            tok0 = qt * 512
            bq = qt % 2
            for m in (0, 1):
                rows = slice(64 * m, 64 * m + 64)
                self.dma("sp", Qz[m][bq], Qz[m][bq].ap[rows, :, 0:n],
                         self.QT_d.rearrange("h p t -> p h t")[rows, :, tok0:tok0 + n],
                         [self.QT_r[h][qt] for h in range(NH)], [Qz[m][bq].r])
            pj = 2 if qt == 8 else (0 if qt < 4 else 1)
            self.dma("sp", CVt[bq], CVt[bq].ap[:, :, 0:n], self.CV_d.rearrange("c p t -> p c t")[:, :, tok0:tok0 + n],
                     [self.CV_r[c][pj] for c in range(4)], [CVt[bq].r])

        load_q(0)
        for qt in range(nqt):
            ctxq = qt == 8
            tok0 = qt * 512
            n = 256 if ctxq else 512
            chunks = [32, 33] if ctxq else list(range(NT))
            jmod = 2 if ctxq else b
            bq = qt % 2
            if qt + 1 < nqt:
                load_q(qt + 1)
            CV, A = CVt[bq], AT[bq]
            pending = None
            SCB = [[PB[0], PB[1]], [PB[2], PB[3]]]
            PVB = [PB[4], PB[5]]
            SM0 = PB[6]
            MISC = PB[7]
            nch = len(chunks)
            steps = [chunks[i:i + 2] for i in range(0, nch, 2)]
            nsteps = len(steps)

            def emit_scores(h, hb, si, ets):
                for j, kc in enumerate(steps[si]):
                    for m in (0, 1):
                        q = Qz[m][bq]
                        self.mm(SCB[m][j].ap[:, 0:n], KT.ap[:, h, kc * P:(kc + 1) * P], q.ap[:, h, 0:n], True, True,
                                [KT.r, q.r], [SCB[m][j].r])
                for j, kc in enumerate(steps[si]):
                    ci = 2 * si + j
                    for m in (0, 1):
                        et = ET[m][ecnt[m] % 4]
                        ecnt[m] += 1
                        self.act(et.ap[:, 0:n], SCB[m][j].ap[:, 0:n], AF.Exp, [SCB[m][j].r], [et.r], scale=QSCALE)
                        if m == 1:
                            ea = EA1[hb]
                            if ci == 0:
                                self.cp("dve", ea.ap[:, 0:n], et.ap[:, 0:n], [et.r], [ea.r])
                            else:
                                self.tt("dve", ea.ap[:, 0:n], ea.ap[:, 0:n], et.ap[:, 0:n], ALU.add, [ea.r, et.r], [ea.r])
                        ets[(si, j, m)] = et

            def epi_a(h, hb):
                self.S.op("dve", (lambda e, n=n: e.reciprocal(out=r12[0].ap[:, 0:n], in_=S0c.ap[:, 0:n])),
                          reads=[S0c.r], writes=[r12[0].r])
                self.tt("dve", a12[0].ap[:, 0:n], O12[0].ap[:, 0:n], r12[0].ap[:, 0:n], ALU.mult,
                        [O12[0].r, r12[0].r], [a12[0].r])

            def epi_b(h, hb):
                self.mm(MISC.ap[:, 0:n], self.onesf.ap, EA1[hb].ap[:, 0:n], True, True,
                        [self.onesf.r, EA1[hb].r], [MISC.r])
                self.S.op("dve", (lambda e, n=n: e.reciprocal(out=r12[1].ap[:, 0:n], in_=MISC.ap[:, 0:n])),
                          reads=[MISC.r], writes=[r12[1].r])
                self.tt("dve", a12[1].ap[:, 0:n], O12[1].ap[:, 0:n], r12[1].ap[:, 0:n], ALU.mult,
                        [O12[1].r, r12[1].r], [a12[1].r])
                self.stt("dve", vv[hb].ap[:, 0:n], a12[1].ap[:, 0:n], self.nlam.ap[:, l:l + 1], a12[0].ap[:, 0:n],
                         ALU.mult, ALU.add, [a12[0].r, a12[1].r, self.nlam.r], [vv[hb].r])
                self.tt("pool", sq[hb].ap[:, 0:n], vv[hb].ap[:, 0:n], vv[hb].ap[:, 0:n], ALU.mult, [vv[hb].r], [sq[hb].r])

            def tail(h, hb):
                self.mm(MISC.ap[:, 0:n], self.onesf.ap, sq[hb].ap[:, 0:n], True, True, [self.onesf.r, sq[hb].r], [MISC.r])
                self.act(lnr[hb].ap[:, 0:n], MISC.ap[:, 0:n], AF.Ln, [MISC.r], [lnr[hb].r], scale=1.0 / P, bias=HEAD_EPS)
                self.act(lnr[hb].ap[:, 0:n], lnr[hb].ap[:, 0:n], AF.Exp, [lnr[hb].r], [lnr[hb].r], scale=-0.5)
                self.stt("dve", A.ap[:, h, 0:n], vv[hb].ap[:, 0:n], self.gsc.ap[:, l, h:h + 1], lnr[hb].ap[:, 0:n],
                         ALU.mult, ALU.mult, [vv[hb].r, self.gsc.r, lnr[hb].r], [A.r])

            def flush(pend):
                stage = pend[2]
                if stage <= 0:
                    epi_a(pend[0], pend[1])
                if stage <= 1:
                    epi_b(pend[0], pend[1])
                tail(pend[0], pend[1])

            for h in range(NH):
                hb = hcnt % 2
                hcnt += 1
                if pending is not None and nsteps < 8:
                    flush(pending)
                    pending = None
                ets = {}
                emit_scores(h, hb, 0, ets)
                for si in range(nsteps):
                    if si + 1 < nsteps:
                        emit_scores(h, hb, si + 1, ets)
                    for m in (0, 1):
                        for j, kc in enumerate(steps[si]):
                            ci = 2 * si + j
                            self.mm(PVB[m].ap[:, 0:n], V.ap[:, kc, h * P:(h + 1) * P], ets[(si, j, m)].ap[:, 0:n], ci == 0,
                                    ci == nch - 1, [V.r, ets[(si, j, m)].r], [PVB[m].r])
                    for j, kc in enumerate(steps[si]):
                        ci = 2 * si + j
                        self.mm(SM0.ap[:, 0:n], self.onesb.ap, ets[(si, j, 0)].ap[:, 0:n], ci == 0, ci == nch - 1,
                                [self.onesb.r, ets[(si, j, 0)].r], [SM0.r])
                    if pending is not None:
                        if si == 1 and pending[2] == 0:
                            epi_a(pending[0], pending[1])
                            pending = (pending[0], pending[1], 1)
                        elif si == 3 and pending[2] == 1:
                            epi_b(pending[0], pending[1])
                            pending = (pending[0], pending[1], 2)
                        elif si == 6 and pending[2] == 2:
                            tail(pending[0], pending[1])
                            pending = None
                for m in (0, 1):
                    self.cp("dve", O12[m].ap[:, 0:n], PVB[m].ap[:, 0:n], [PVB[m].r], [O12[m].r])
                self.cp("dve", S0c.ap[:, 0:n], SM0.ap[:, 0:n], [SM0.r], [S0c.r])
                pending = (h, hb, 0)
            flush(pending)
            pending = None
            if self.stop_after == "att_q0":
                for nm, bf, nf in (("A", A, 2048), ("vv", vv[1], 512), ("lnr", lnr[1], 512), ("sq", sq[1], 512),
                                   ("a0", a12[0], 512), ("a1", a12[1], 512), ("r0", r12[0], 512), ("r1", r12[1], 512)):
                    if nm in self.dbg_out:
                        self.dbg_dump_bf16(nm, bf, nf)
                return
            for s in range(n // P):
                i = tok0 // P + s
                zi = scnt % 2
                scnt += 1
                src, src_r = self.xsrc(first, b, i)
                self.dma("sp", xt[zi], xt[zi].ap, src, [src_r] if src_r is not None else [], [xt[zi].r])
                for half in (0, 1):
                    bank = PB[6 + (ocnt % 2)]
                    ocnt += 1
                    for k in range(KD):
                        lhsT = A.ap[:, k, s * P:(s + 1) * P] if k < 4 else CV.ap[:, k - 4, s * P:(s + 1) * P]
                        self.mm(bank.ap, lhsT, WO.ap[:, k, half * 512:(half + 1) * 512], k == 0, k == KD - 1,
                                [A.r if k < 4 else CV.r, WO.r], [bank.r])
                    self.tt("dve", z[zi].ap[:, half * 512:(half + 1) * 512], bank.ap,
                            g1bc[jmod].ap[:, half * 512:(half + 1) * 512], ALU.mult, [bank.r, g1bc[jmod].r], [z[zi].r])
                self.stt("dve", z[zi].ap, xt[zi].ap, ALPHA, z[zi].ap, ALU.mult, ALU.add, [xt[zi].r, z[zi].r], [z[zi].r])
                self.post_ln(z[zi], self.stat[zi], zn[zi], oo[zi], lng, lnb, self.xs_d[b, i * P:(i + 1) * P, :],
                             self.xs_r[b][i])
            if self.stop_after == "att_q0b" or (self.stop_after == "att_q1b" and qt == 1):
                self.dbg_dump_bf16("A", AT[0], 2048)
                return

    def moe_phase(self, l, b, last):
        nsub = 32 if last else NT
        sizes = [(nsub + 2 - k) // 3 for k in range(3)]
        bounds = []
        s0 = 0
        for sz in sizes:
            bounds.append((s0, s0 + sz))
            s0 += sz
        for (i0, i1) in bounds:
            self.moe_pass(l, b, last, i0, i1)

    def moe_pass(self, l, b, last, i0, i1):
        self.phase_begin()
        PB = self.PB
        ns = i1 - i0
        ntok = ns * P
        HT = self.aalloc("HT", [KD, 12 * P], BF16)
        ACC = self.aalloc("ACC", [12, D], F32)
        acc_r = [Res("acc%d" % i) for i in range(ns)]
        gates = self.aalloc("gates", [12, NE], F32)
        WG = [self.aalloc("WG%d" % i, [KD, DEXP], BF16, stream=True) for i in range(2)]
        WU = [self.aalloc("WU%d" % i, [KD, DEXP], BF16, stream=True) for i in range(2)]
        WD = [self.aalloc("WD%d" % i, [4, D], BF16, stream=True) for i in range(2)]
        HE = [self.aalloc("HE%d" % i, [4, 512], BF16) for i in range(2)]
        SG = [self.aalloc("SG%d" % i, [512], F32) for i in range(2)]
        js = sorted(set((b if i < 32 else 2) for i in range(i0, i1)))
        g2bc = {}
        for j in js:
            g2bc[j] = self.aalloc("g2bc%d" % j, [D], F32, stream=True)
            self.dma("sp", g2bc[j], g2bc[j].ap, self.mrow_d[l, j:j + 1, 5 * D:6 * D].broadcast_to([P, D]),
                     [self.mrow_r[l]], [g2bc[j].r])
        lng = self.aalloc("lng2", [D], F32, stream=True)
        lnb = self.aalloc("lnb2", [D], F32, stream=True)
        self.dma("sp", lng, lng.ap, self.ln2_g[l:l + 1, :].broadcast_to([P, D]), [], [lng.r])
        self.dma("sp", lnb, lnb.ap, self.ln2_b[l:l + 1, :].broadcast_to([P, D]), [], [lnb.r])
        self.lnt_setup()
        z = [self.aalloc("mz%d" % i, [D], F32) for i in range(3)]
        zn = z
        oo = [self.aalloc("mo%d" % i, [D], F32, stream=True) for i in range(3)]

        def load_expert(e):
            bi = e % 2
            self.dma("pool", WG[bi], WG[bi].ap, self.w_gate[l, e].rearrange("(k p) n -> p k n", p=P), [], [WG[bi].r])
            self.dma("pool", WU[bi], WU[bi].ap, self.w_up[l, e].rearrange("(k p) n -> p k n", p=P), [], [WU[bi].r])
            self.dma("pool", WD[bi], WD[bi].ap, self.w_down[l, e].rearrange("(k p) n -> p k n", p=P), [], [WD[bi].r])

        load_expert(0)
        for ii in range(ns):
            i = i0 + ii
            j = b if i < 32 else 2
            self.lnt_tile(self.xs_d[b, i * P:(i + 1) * P, :], self.xs_r[b][i], HT, ii * P,
                          self.m1p.ap[:, l, 1, :, j], self.mT.ap[:, l, 24:32, j])
            pl = PB[6]
            for k in range(KD):
                self.mm(pl.ap[:, 0:NE], HT.ap[:, k, ii * P:(ii + 1) * P], self.rwb.ap[:, k, :], k == 0, k == KD - 1,
                        [HT.r, self.rwb.r], [pl.r])
            self.router(pl, gates, ii)
        tiles = []
        t0 = 0
        while t0 < ntok:
            n = min(512, ntok - t0)
            tiles.append((t0, n))
            t0 += n
        items = [(e, t0, n) for e in range(NE) for (t0, n) in tiles]
        ycnt = [0]

        def gu(idx):
            e, t0, n = items[idx]
            bi = e % 2
            he = HE[idx % 2]
            for c in range(4):
                pg, pu = PB[2 * (c % 2)], PB[2 * (c % 2) + 1]
                for k in range(KD):
                    self.mm(pg.ap[:, 0:n], WG[bi].ap[:, k, c * P:(c + 1) * P], HT.ap[:, k, t0:t0 + n], k == 0, k == KD - 1,
                            [WG[bi].r, HT.r], [pg.r])
                for k in range(KD):
                    self.mm(pu.ap[:, 0:n], WU[bi].ap[:, k, c * P:(c + 1) * P], HT.ap[:, k, t0:t0 + n], k == 0, k == KD - 1,
                            [WU[bi].r, HT.r], [pu.r])
                sg = SG[c % 2]
                self.act(sg.ap[:, 0:n], pg.ap[:, 0:n], AF.Silu, [pg.r], [sg.r])
                self.tt("dve", he.ap[:, c, 0:n], sg.ap[:, 0:n], pu.ap[:, 0:n], ALU.mult, [sg.r, pu.r], [he.r])

        def down(idx):
            e, t0, n = items[idx]
            bi = e % 2
            he = HE[idx % 2]
            for s in range(n // P):
                ii = t0 // P + s
                for half in (0, 1):
                    py = PB[4 + (ycnt[0] % 2)]
                    ycnt[0] += 1
                    for c in range(4):
                        self.mm(py.ap, he.ap[:, c, s * P:(s + 1) * P], WD[bi].ap[:, c, half * 512:(half + 1) * 512],
                                c == 0, c == 3, [he.r, WD[bi].r], [py.r])
                    dst = ACC.ap[:, ii, half * 512:(half + 1) * 512]
                    g = gates.ap[:, ii, e:e + 1]
                    if e == 0:
                        self.ts("dve", dst, py.ap, g, None, ALU.mult, None, [py.r, gates.r], [acc_r[ii]])
                    else:
                        self.stt("dve", dst, py.ap, g, dst, ALU.mult, ALU.add, [py.r, gates.r, acc_r[ii]], [acc_r[ii]])

        for idx in range(len(items)):
            e, t0, n = items[idx]
            if t0 == 0 and e + 1 < NE:
                load_expert(e + 1)
            if idx == 0:
                gu(0)
            if idx + 1 < len(items):
                gu(idx + 1)
            down(idx)
        for ii in range(ns):
            i = i0 + ii
            j = b if i < 32 else 2
            zi = ii % 3
            xt = self.lnt_xt[zi]
            self.dma("sp", xt, xt.ap, self.xs_d[b, i * P:(i + 1) * P, :], [self.xs_r[b][i]], [xt.r])
            self.tt("pool", z[zi].ap, ACC.ap[:, ii, :], g2bc[j].ap, ALU.mult, [acc_r[ii], g2bc[j].r], [z[zi].r])
            self.stt("dve", z[zi].ap, xt.ap, ALPHA, z[zi].ap, ALU.mult, ALU.add, [xt.r, z[zi].r], [z[zi].r])
            if last:
                dst, dst_r = self.y[b, i * P:(i + 1) * P, :], self.y_r
            else:
                dst, dst_r = self.xs_d[b, i * P:(i + 1) * P, :], self.xs_r[b][i]
            self.post_ln(z[zi], self.stat[zi], zn[zi], oo[zi], lng, lnb, dst, dst_r)

    def router(self, pl, gates, ii):
        t = self.rt[ii % 2]
        R = [t.r]
        a = t.ap
        sc, sel, m1, sel2, m2, gs, gm, ing, msk, oh, w = (a[:, 16 * i:16 * (i + 1)] for i in range(11))
        den = a[:, 176:177]
        self.act(sc, pl.ap[:, 0:NE], AF.Exp, [pl.r], R, scale=-1.0)
        self.ts("dve", sc, sc, 1.0, None, ALU.add, None, R, R)
        self.S.op("dve", lambda e: e.reciprocal(out=sc, in_=sc), reads=R, writes=R) if False else \
            self.S.op("dve", lambda e: e.reciprocal(out=sc, in_=sc), reads=R, writes=R)
        self.tt("dve", sel, sc, self.rbias.ap, ALU.add, R + [self.rbias.r], R)
        g4 = lambda v: v.rearrange("p (g e) -> p g e", g=4)
        self.red("dve", m1[:, 0:4], g4(sel), ALU.max, R, R)
        self.tt("dve", g4(sel2), g4(sel), m1[:, 0:4].unsqueeze(2).broadcast_to([P, 4, 4]), ALU.is_equal, R, R)
        self.stt("dve", sel2, sel2, -BIG, sel, ALU.mult, ALU.add, R, R)
        self.red("dve", m2[:, 0:4], g4(sel2), ALU.max, R, R)
        self.tt("dve", gs[:, 0:4], m1[:, 0:4], m2[:, 0:4], ALU.add, R, R)
        self.red("dve", gm[:, 0:1], gs[:, 0:4], ALU.max, R, R)
        self.ts("dve", ing[:, 0:4], gs[:, 0:4], gm[:, 0:1], None, ALU.is_equal, None, R, R)
        self.ts("dve", ing[:, 0:4], ing[:, 0:4], -1.0, BIG, ALU.add, ALU.mult, R, R)
        self.tt("dve", g4(msk), g4(sel), ing[:, 0:4].unsqueeze(2).broadcast_to([P, 4, 4]), ALU.add, R, R)
        self.red("dve", gm[:, 1:2], msk, ALU.max, R, R)
        self.ts("dve", oh, msk, gm[:, 1:2], None, ALU.is_equal, None, R, R)
        self.stt("dve", msk, oh, -BIG, msk, ALU.mult, ALU.add, R, R)
        self.red("dve", gm[:, 2:3], msk, ALU.max, R, R)
        self.ts("dve", sel2, msk, gm[:, 2:3], None, ALU.is_equal, None, R, R)
        self.tt("dve", oh, oh, sel2, ALU.add, R, R)
        self.tt("dve", w, sc, oh, ALU.mult, R, R)
        self.red("dve", den, w, ALU.add, R, R)
        self.S.op("dve", lambda e: e.reciprocal(out=den, in_=den), reads=R, writes=R)
        self.ts("dve", gates.ap[:, ii, :], w, den, None, ALU.mult, None, R, [gates.r])

    debug_layer = 0

    def dbg_dump_bf16(self, name, buf, nfree):
        self.S.barrier()
        out = self.dbg_out[name]
        tmp = self.aalloc("dbgtmp_" + name, [512], F32)
        flat = buf.ap
        if len(flat.shape) == 3:
            flat = flat.rearrange("p a b -> p (a b)")
        for o in range(0, nfree, 512):
            n = min(512, nfree - o)
            self.cp("dve", tmp.ap[:, 0:n], flat[:, o:o + n], [buf.r], [tmp.r])
            self.dma("sp", self.dbg_stream, out[:, o:o + n], tmp.ap[:, 0:n], [tmp.r], [self.dbg_r])
        self.S.barrier()


def _rope_tables():
    t = np.arange(NLAT, dtype=np.int32)
    row = (t // 64).astype(np.float32)
    col = (t % 64).astype(np.float32)
    inv = (np.float32(1.0) / (np.float32(10000.0) ** (np.arange(16, dtype=np.float32) / np.float32(16)))).astype(np.float32)
    cosT = np.zeros((P, NLAT), np.float32)
    sinT = np.zeros((P, NLAT), np.float32)
    for p in range(P):
        f = p % 64
        axis, half, fr = f // 32, (f // 16) % 2, f % 16
        pos = row if axis == 0 else col
        ang = (pos * inv[fr]).astype(np.float32)
        cosT[p] = np.cos(ang)
        sinT[p] = np.sin(ang) * (-1.0 if half == 0 else 1.0)
    return cosT, sinT


def _host_inputs(inputs):
    f = lambda k: np.ascontiguousarray(np.asarray(inputs[k], dtype=np.float32))
    w_in = f("w_in")
    cols = np.arange(512)
    perm = cols ^ 16
    q, k_ = w_in[:, :, 0:512], w_in[:, :, 512:1024]
    w_inx = np.ascontiguousarray(np.concatenate(
        [q, q[:, :, perm], k_, k_[:, :, perm], w_in[:, :, 1024:1536], w_in[:, :, 1536:2048], w_in[:, :, 2048:2560],
         w_in[:, :, 2560:3072]], axis=2))
    cosT, sinT = _rope_tables()
    shared = {
        "w_mod": f("w_mod"), "b_mod": f("b_mod"), "w_inx": w_inx,
        "dl": np.ascontiguousarray(f("diff_lambda").reshape(1, -1)),
        "gT": np.ascontiguousarray(f("attn_norm_g").reshape(DEPTH, NH, P).transpose(2, 0, 1)),
        "cwT": np.ascontiguousarray(f("conv_w").reshape(DEPTH, 3, 4, P).transpose(3, 0, 1, 2)),
        "cbT": np.ascontiguousarray(f("conv_b").reshape(DEPTH, 4, P).transpose(2, 0, 1)),
        "w_out": f("w_out"), "ln1_g": f("ln1_g"), "ln1_b": f("ln1_b"), "ln2_g": f("ln2_g"), "ln2_b": f("ln2_b"),
        "router_w": f("router_w"), "router_bias": np.ascontiguousarray(f("router_bias").reshape(1, NE)),
        "w_gate": f("w_gate"), "w_up": f("w_up"), "w_down": f("w_down"),
        "ident": np.eye(P, dtype=np.float32), "cosT": cosT, "sinT": sinT,
    }
    x, c, ctx, c_ctx = f("x"), f("c"), f("ctx"), f("c_ctx")
    in_maps = []
    for i in range(NCORES):
        cc = np.stack([c[2 * i], c[2 * i + 1], c_ctx], axis=0)
        ccT = np.ascontiguousarray(cc.reshape(3, KD, P).transpose(2, 1, 0))
        m = dict(shared)
        m["x"] = np.ascontiguousarray(x[2 * i:2 * i + 2])
        m["ctx"] = np.ascontiguousarray(ctx[2 * i:2 * i + 2])
        m["ccT"] = ccT
        in_maps.append(m)
    return in_maps


_NC_CACHE = {}


def kernel(**inputs):
    in_maps = _host_inputs(inputs)
    if "nc" not in _NC_CACHE:
        _NC_CACHE["nc"] = Builder().build()
    res = run_bass_kernel_spmd(_NC_CACHE["nc"], in_maps, core_ids=list(range(NCORES)))
    return np.concatenate([np.asarray(r["y"], dtype=np.float32) for r in res.results], axis=0)
```
